# Optimizing a Trainium2 kernel written in Bass

```python
import math
import jax
import jax.numpy as jnp
from jax import lax
import numpy as np

D_MODEL = 2048
BATCH = 8
SEQ = 4096
DEPTH = 2

D_RET = 3 * D_MODEL // 8
D_MLSTM = 3 * D_MODEL // 8
D_S5 = D_MODEL // 4
D_MIX = D_RET + D_MLSTM + D_S5
RET_V_DIM = 128
RET_QK_DIM = 64
RET_HEADS = D_RET // RET_V_DIM
MLSTM_DIM = 128
MLSTM_HEADS = D_MLSTM // MLSTM_DIM
MLSTM_CONV = 4
S5_GROUP_CH = 16
S5_GROUPS = D_S5 // S5_GROUP_CH
S5_STATE = 64
D_FF = 5632
FFN_CONV = 3
CHUNK = 128
ROPE_BASE = 10000.0
EPS = 1e-6
D_IN = 2 * RET_HEADS * RET_QK_DIM + 2 * D_RET + 4 * D_MLSTM + 2 * MLSTM_HEADS + D_S5

kernel_name = "hybrid_retention_mlstm_s5_convffn"


def rmsnorm(x, w):
    xf = x.astype(jnp.float32)
    y = xf * lax.rsqrt(jnp.mean(xf * xf, axis=-1, keepdims=True) + EPS)
    return (y * w.astype(jnp.float32)).astype(x.dtype)


def head_groupnorm(h, w):
    b, t, nh, dh = h.shape
    hf = h.astype(jnp.float32)
    mu = jnp.mean(hf, axis=-1, keepdims=True)
    var = jnp.mean(jnp.square(hf - mu), axis=-1, keepdims=True)
    y = ((hf - mu) * lax.rsqrt(var + EPS)).reshape(b, t, nh * dh)
    return (y * w.astype(jnp.float32)).astype(h.dtype)


def causal_dwconv(x, w, b):
    k = w.shape[0]
    y = lax.conv_general_dilated(x, w[:, None, :], window_strides=(1,), padding=[(k - 1, 0)],
                                 dimension_numbers=("NWC", "WIO", "NWC"),
                                 feature_group_count=x.shape[-1])
    return y + b


def rotary(x):
    t, dh = x.shape[1], x.shape[-1]
    inv = ROPE_BASE ** (-jnp.arange(0, dh, 2, dtype=jnp.float32) / dh)
    ang = jnp.arange(t, dtype=jnp.float32)[:, None] * inv[None, :]
    cos = jnp.cos(ang)[None, :, None, :].astype(x.dtype)
    sin = jnp.sin(ang)[None, :, None, :].astype(x.dtype)
    x1, x2 = jnp.split(x, 2, axis=-1)
    return jnp.concatenate([x1 * cos - x2 * sin, x1 * sin + x2 * cos], axis=-1)


def to_chunks(a):
    b, t, nh, d = a.shape
    return a.reshape(b, t // CHUNK, CHUNK, nh, d).transpose(1, 0, 3, 2, 4)


def from_chunks(a):
    n, b, nh, l, d = a.shape
    return a.transpose(1, 0, 3, 2, 4).reshape(b, n * l, nh, d)


def retention(q, k, v):
    b, t, nh, dk = q.shape
    dv = v.shape[-1]
    dt = q.dtype
    log_gamma = jnp.log1p(-jnp.exp2(-5.0 - jnp.arange(nh, dtype=jnp.float32)))
    idx = jnp.arange(CHUNK, dtype=jnp.float32)
    diff = idx[:, None] - idx[None, :]
    intra = jnp.where(diff[None] >= 0, jnp.exp(jnp.maximum(diff, 0.0)[None] * log_gamma[:, None, None]), 0.0).astype(dt)
    q_decay = jnp.exp((idx[None, :] + 1.0) * log_gamma[:, None])[None, :, :, None].astype(dt)
    k_decay = jnp.exp((CHUNK - 1.0 - idx)[None, :] * log_gamma[:, None])[None, :, :, None].astype(dt)
    chunk_decay = jnp.exp(CHUNK * log_gamma)[None, :, None, None].astype(dt)
    q = q * (dk ** -0.5)

    def step(state, inp):
        qi, ki, vi = inp
        s = jnp.einsum('bhld,bhmd->bhlm', qi, ki) * intra
        inner = jnp.einsum('bhlm,bhmv->bhlv', s, vi)
        cross = jnp.einsum('bhld,bhdv->bhlv', qi, state) * q_decay
        state = state * chunk_decay + jnp.einsum('bhld,bhlv->bhdv', ki * k_decay, vi)
        return state, inner + cross

    state0 = jnp.zeros((b, nh, dk, dv), dt)
    _, out = lax.scan(step, state0, (to_chunks(q), to_chunks(k), to_chunks(v)))
    return from_chunks(out)


def mlstm(q, k, v, i_pre, f_pre):
    b, t, nh, d = q.shape
    f32 = jnp.float32
    qf = q.astype(f32)
    kf = k.astype(f32) * (d ** -0.5)
    vf = v.astype(f32)
    ig = i_pre.astype(f32)
    logf = jax.nn.log_sigmoid(f_pre.astype(f32))
    gate_chunks = lambda a: a.reshape(b, t // CHUNK, CHUNK, nh).transpose(1, 0, 3, 2)
    causal = jnp.tril(jnp.ones((CHUNK, CHUNK), dtype=bool))

    def step(carry, inp):
        c_st, n_st, m_st = carry
        qi, ki, vi, ii, lf = inp
        cum = jnp.cumsum(lf, axis=-1)
        logw = jnp.where(causal, cum[..., :, None] - cum[..., None, :] + ii[..., None, :], -jnp.inf)
        inter = cum + m_st[..., None]
        m_t = jnp.maximum(inter, jnp.max(logw, axis=-1))
        w = jnp.exp(logw - m_t[..., None])
        sc = jnp.exp(inter - m_t)
        qk = jnp.einsum('bhld,bhsd->bhls', qi, ki) * w
        num = jnp.einsum('bhls,bhsd->bhld', qk, vi) + sc[..., None] * jnp.einsum('bhld,bhde->bhle', qi, c_st)
        den = jnp.sum(qk, axis=-1) + sc * jnp.einsum('bhld,bhd->bhl', qi, n_st)
        h = num / jnp.maximum(jnp.abs(den), jnp.exp(-m_t))[..., None]
        last = cum[..., -1]
        logw_end = last[..., None] - cum + ii
        m_new = jnp.maximum(last + m_st, jnp.max(logw_end, axis=-1))
        kw = ki * jnp.exp(logw_end - m_new[..., None])[..., None]
        decay = jnp.exp(last + m_st - m_new)
        c_new = decay[..., None, None] * c_st + jnp.einsum('bhsd,bhse->bhde', kw, vi)
        n_new = decay[..., None] * n_st + jnp.sum(kw, axis=-2)
        return (c_new, n_new, m_new), h

    carry0 = (jnp.zeros((b, nh, d, d), f32), jnp.zeros((b, nh, d), f32), jnp.zeros((b, nh), f32))
    _, hs = lax.scan(step, carry0, (to_chunks(qf), to_chunks(kf), to_chunks(vf), gate_chunks(ig), gate_chunks(logf)))
    return from_chunks(hs).astype(q.dtype)


def s5(u, a_re, a_im, log_step, b_re, b_im, c_re, c_im, d_skip, glu_w, glu_b):
    bsz, t, _ = u.shape
    f32 = jnp.float32
    uf = u.astype(f32).reshape(bsz, t, S5_GROUPS, S5_GROUP_CH)
    ar, ai = a_re.astype(f32), a_im.astype(f32)
    step = jnp.exp(log_step.astype(f32))[:, None]
    mag = jnp.exp(ar * step)
    abar_re, abar_im = mag * jnp.cos(ai * step), mag * jnp.sin(ai * step)
    xr, xi = abar_re - 1.0, abar_im
    den = ar * ar + ai * ai
    fr, fi = (xr * ar + xi * ai) / den, (xi * ar - xr * ai) / den
    br, bi = b_re.astype(f32), b_im.astype(f32)
    bbar_re = fr[..., None] * br - fi[..., None] * bi
    bbar_im = fr[..., None] * bi + fi[..., None] * br
    bu_re = jnp.einsum('btgc,gpc->btgp', uf, bbar_re)
    bu_im = jnp.einsum('btgc,gpc->btgp', uf, bbar_im)
    at_re = jnp.broadcast_to(abar_re, (1, t, S5_GROUPS, S5_STATE))
    at_im = jnp.broadcast_to(abar_im, (1, t, S5_GROUPS, S5_STATE))

    def combine(e1, e2):
        a1r, a1i, b1r, b1i = e1
        a2r, a2i, b2r, b2i = e2
        return (a2r * a1r - a2i * a1i, a2r * a1i + a2i * a1r,
                a2r * b1r - a2i * b1i + b2r, a2r * b1i + a2i * b1r + b2i)

    _, _, s_re, s_im = lax.associative_scan(combine, (at_re, at_im, bu_re, bu_im), axis=1)
    y = (jnp.einsum('btgp,gcp->btgc', s_re, c_re.astype(f32))
         - jnp.einsum('btgp,gcp->btgc', s_im, c_im.astype(f32))
         + d_skip.astype(f32).reshape(S5_GROUPS, S5_GROUP_CH) * uf).reshape(bsz, t, D_S5)
    g = jax.nn.gelu(y)
    out = g * jax.nn.sigmoid(g @ glu_w.astype(f32) + glu_b.astype(f32))
    return out.astype(u.dtype)


def token_mixer(h, w_in, mlstm_conv_w, mlstm_conv_b, mlstm_gate_b, ret_gn_w, mlstm_gn_w,
                s5_A_re, s5_A_im, s5_log_step, s5_B_re, s5_B_im, s5_C_re, s5_C_im, s5_D,
                s5_glu_w, s5_glu_b, w_out):
    bsz, t, _ = h.shape
    sizes = (RET_HEADS * RET_QK_DIM, RET_HEADS * RET_QK_DIM, D_RET, D_RET,
             2 * D_MLSTM, D_MLSTM, D_MLSTM, 2 * MLSTM_HEADS, D_S5)
    splits = np.cumsum(sizes)[:-1].tolist()
    proj = h @ w_in
    r_q, r_k, r_v, r_g, m_qk, m_v, m_o, m_gates, s_u = jnp.split(proj, splits, axis=-1)
    rq = rotary(r_q.reshape(bsz, t, RET_HEADS, RET_QK_DIM))
    rk = rotary(r_k.reshape(bsz, t, RET_HEADS, RET_QK_DIM))
    ret = retention(rq, rk, r_v.reshape(bsz, t, RET_HEADS, RET_V_DIM))
    ret_out = jax.nn.silu(r_g) * head_groupnorm(ret, ret_gn_w)
    m_qk = jax.nn.silu(causal_dwconv(m_qk, mlstm_conv_w, mlstm_conv_b))
    mq, mk = jnp.split(m_qk, 2, axis=-1)
    i_pre, f_pre = jnp.split(m_gates + mlstm_gate_b, 2, axis=-1)
    hm = mlstm(mq.reshape(bsz, t, MLSTM_HEADS, MLSTM_DIM), mk.reshape(bsz, t, MLSTM_HEADS, MLSTM_DIM),
               m_v.reshape(bsz, t, MLSTM_HEADS, MLSTM_DIM), i_pre, f_pre)
    mlstm_out = jax.nn.sigmoid(m_o) * head_groupnorm(hm, mlstm_gn_w)
    s5_out = s5(s_u, s5_A_re, s5_A_im, s5_log_step, s5_B_re, s5_B_im, s5_C_re, s5_C_im, s5_D,
                s5_glu_w, s5_glu_b)
    return jnp.concatenate([ret_out, mlstm_out, s5_out], axis=-1) @ w_out


def conv_ffn(h, w_up, conv_w, conv_b, w_down):
    up = causal_dwconv(h @ w_up, conv_w, conv_b)
    val, gate = jnp.split(up, 2, axis=-1)
    return (jax.nn.silu(gate) * val) @ w_down


def setup_inputs(seed: int = 0) -> dict:
    key = jax.random.key(seed)
    ks = jax.random.split(key, 32)
    f32 = jnp.float32
    nrm = lambda k, shape, scale: jax.random.normal(k, shape, f32) * scale
    x = nrm(ks[0], (BATCH, SEQ, D_MODEL), 1.0)
    norm1_w = 1.0 + nrm(ks[1], (DEPTH, D_MODEL), 0.02)
    w_in = nrm(ks[2], (DEPTH, D_MODEL, D_IN), D_MODEL ** -0.5)
    mlstm_conv_w = nrm(ks[3], (DEPTH, MLSTM_CONV, 2 * D_MLSTM), MLSTM_CONV ** -0.5)
    mlstm_conv_b = nrm(ks[4], (DEPTH, 2 * D_MLSTM), 0.01)
    i_bias = nrm(ks[5], (DEPTH, MLSTM_HEADS), 0.1)
    f_bias = jnp.linspace(3.0, 6.0, MLSTM_HEADS, dtype=f32)[None, :] + nrm(ks[6], (DEPTH, MLSTM_HEADS), 0.1)
    mlstm_gate_b = jnp.concatenate([i_bias, f_bias], axis=-1)
    ret_gn_w = 1.0 + nrm(ks[7], (DEPTH, D_RET), 0.02)
    mlstm_gn_w = 1.0 + nrm(ks[8], (DEPTH, D_MLSTM), 0.02)
    n_idx = jnp.arange(S5_STATE, dtype=f32)
    s5_A_re = -0.5 + nrm(ks[9], (DEPTH, S5_GROUPS, S5_STATE), 0.01)
    s5_A_im = jnp.pi * n_idx + nrm(ks[10], (DEPTH, S5_GROUPS, S5_STATE), 0.01)
    s5_log_step = jax.random.uniform(ks[11], (DEPTH, S5_GROUPS), f32, math.log(0.001), math.log(0.1))
    s5_B_re = nrm(ks[12], (DEPTH, S5_GROUPS, S5_STATE, S5_GROUP_CH), (2 * S5_GROUP_CH) ** -0.5)
    s5_B_im = nrm(ks[13], (DEPTH, S5_GROUPS, S5_STATE, S5_GROUP_CH), (2 * S5_GROUP_CH) ** -0.5)
    s5_C_re = nrm(ks[14], (DEPTH, S5_GROUPS, S5_GROUP_CH, S5_STATE), S5_STATE ** -0.5)
    s5_C_im = nrm(ks[15], (DEPTH, S5_GROUPS, S5_GROUP_CH, S5_STATE), S5_STATE ** -0.5)
    s5_D = nrm(ks[16], (DEPTH, D_S5), 1.0)
    s5_glu_w = nrm(ks[17], (DEPTH, D_S5, D_S5), D_S5 ** -0.5)
    s5_glu_b = nrm(ks[18], (DEPTH, D_S5), 0.01)
    w_out = nrm(ks[19], (DEPTH, D_MIX, D_MODEL), D_MIX ** -0.5)
    norm2_w = 1.0 + nrm(ks[20], (DEPTH, D_MODEL), 0.02)
    ffn_w_up = nrm(ks[21], (DEPTH, D_MODEL, 2 * D_FF), D_MODEL ** -0.5)
    ffn_conv_w = nrm(ks[22], (DEPTH, FFN_CONV, 2 * D_FF), FFN_CONV ** -0.5)
    ffn_conv_b = nrm(ks[23], (DEPTH, 2 * D_FF), 0.01)
    ffn_w_down = nrm(ks[24], (DEPTH, D_FF, D_MODEL), D_FF ** -0.5)
    final_norm_w = 1.0 + nrm(ks[25], (D_MODEL,), 0.02)
    return {"x": x, "norm1_w": norm1_w, "w_in": w_in, "mlstm_conv_w": mlstm_conv_w,
            "mlstm_conv_b": mlstm_conv_b, "mlstm_gate_b": mlstm_gate_b, "ret_gn_w": ret_gn_w,
            "mlstm_gn_w": mlstm_gn_w, "s5_A_re": s5_A_re, "s5_A_im": s5_A_im,
            "s5_log_step": s5_log_step, "s5_B_re": s5_B_re, "s5_B_im": s5_B_im,
            "s5_C_re": s5_C_re, "s5_C_im": s5_C_im, "s5_D": s5_D, "s5_glu_w": s5_glu_w,
            "s5_glu_b": s5_glu_b, "w_out": w_out, "norm2_w": norm2_w, "ffn_w_up": ffn_w_up,
            "ffn_conv_w": ffn_conv_w, "ffn_conv_b": ffn_conv_b, "ffn_w_down": ffn_w_down,
            "final_norm_w": final_norm_w}


def reference(x, norm1_w, w_in, mlstm_conv_w, mlstm_conv_b, mlstm_gate_b, ret_gn_w, mlstm_gn_w,
              s5_A_re, s5_A_im, s5_log_step, s5_B_re, s5_B_im, s5_C_re, s5_C_im, s5_D,
              s5_glu_w, s5_glu_b, w_out, norm2_w, ffn_w_up, ffn_conv_w, ffn_conv_b, ffn_w_down,
              final_norm_w):
    for l in range(DEPTH):
        h = rmsnorm(x, norm1_w[l])
        x = x + token_mixer(h, w_in[l], mlstm_conv_w[l], mlstm_conv_b[l], mlstm_gate_b[l],
                            ret_gn_w[l], mlstm_gn_w[l], s5_A_re[l], s5_A_im[l], s5_log_step[l],
                            s5_B_re[l], s5_B_im[l], s5_C_re[l], s5_C_im[l], s5_D[l],
                            s5_glu_w[l], s5_glu_b[l], w_out[l])
        h = rmsnorm(x, norm2_w[l])
        x = x + conv_ffn(h, ffn_w_up[l], ffn_conv_w[l], ffn_conv_b[l], ffn_w_down[l])
    return rmsnorm(x, final_norm_w)
```

```python
import math
from contextlib import ExitStack
import numpy as np
import concourse.bass as bass
import concourse.mybir as mybir
from concourse.bass_utils import run_bass_kernel_spmd

F32 = mybir.dt.float32
BF16 = mybir.dt.bfloat16
U8 = mybir.dt.uint8
I32 = mybir.dt.int32
AF = mybir.ActivationFunctionType
ALU = mybir.AluOpType
AX = mybir.AxisListType

D = 2048
KT = 16
SEQ = 4096
DEPTH = 2
TT = 512
NCH = 4
D_FF = 5632
NFF = 44
EPS = 1e-6
NQK = 384
D_IN = 5900


class Res:
    __slots__ = ("lw", "rd", "excl")

    def __init__(self, excl=False):
        self.lw = None
        self.rd = {}
        self.excl = excl


class Buf:
    def __init__(self, ap, cells):
        self.ap = ap
        self.cells = cells

    def __getitem__(self, idx):
        return self.ap[idx]


class Op:
    __slots__ = ("eng", "fn", "deps", "dma", "needed", "event")


COMPUTE = ("pe", "act", "dve", "pool")


class _Rec:
    def __getattr__(self, name):
        def f(*a, **k):
            self.call = (name, a, k)
            return self
        return f


class Prog:
    def __init__(self):
        self.ops = []

    def op(self, eng, fn, reads=(), writes=(), dma=False):
        i = len(self.ops)
        o = Op()
        rec = _Rec()
        fn(rec)
        o.eng, o.fn, o.dma, o.needed, o.event = eng, rec.call, dma, False, None
        deps = set()
        rcells = [c for b in reads for c in b.cells]
        wcells = [c for b in writes for c in b.cells]
        raw = set()
        key = ("dma", i) if dma else eng
        for c in rcells:
            if c.lw is not None:
                raw.add(c.lw)
            if c.excl:
                deps.update(v for kk, v in c.rd.items() if kk != key)
        for c in wcells:
            if c.lw is not None:
                deps.add(c.lw)
            deps.update(c.rd.values())
        deps |= raw
        deps.discard(i)
        fl = set()
        for d in deps:
            od = self.ops[d]
            if dma or od.dma or od.eng != eng:
                fl.add(d)
            elif eng != "pe" and d in raw:
                fl.add(d)
        o.deps = fl
        for d in fl:
            self.ops[d].needed = True
        for c in rcells:
            c.rd[key] = i
        for c in wcells:
            c.lw = i
            c.rd = {}
        self.ops.append(o)
        return i

    def emit(self, nc, stack, block_kwargs=None):
        ops = self.ops
        streams = {}
        for i, o in enumerate(ops):
            streams.setdefault(o.eng, []).append(i)
        ROT = 30000
        csem = {}
        ccnt = {e: 0 for e in streams}
        dsem = {}
        dcnt = {}
        NDS = {"sp": 12, "pool": 6, "act": 4}

        def getsem(name):
            return stack.enter_context(nc.semaphore(name))

        for e in streams:
            n_ev = sum(1 for i in streams[e] if ops[i].needed and not ops[i].dma)
            csem[e] = [getsem(f"c_{e}_{k}") for k in range(n_ev // ROT + 1)]
            if any(ops[i].dma for i in streams[e]):
                dsem[e] = [getsem(f"d_{e}_{k}") for k in range(NDS.get(e, 4))]
                dcnt[e] = [0] * len(dsem[e])
        rr = {e: 0 for e in streams}
        prevdma = {}
        for i, o in enumerate(ops):
            if o.dma:
                k = rr[o.eng] % len(dsem[o.eng])
                rr[o.eng] += 1
                dcnt[o.eng][k] += 16
                o.event = (dsem[o.eng][k], dcnt[o.eng][k], ("d", o.eng, k))
            elif o.needed:
                n = ccnt[o.eng]
                ccnt[o.eng] += 1
                o.event = (csem[o.eng][n // ROT], n % ROT + 1, ("c", o.eng, n // ROT))
        engmap = {"pe": "tensor", "act": "scalar", "dve": "vector", "pool": "gpsimd", "sp": "sync"}
        with nc.Block() as block:
            for ename, idxs in streams.items():
                def body(e, idxs=idxs):
                    known = {}
                    for i in idxs:
                        o = ops[i]
                        need = {}
                        for d in o.deps:
                            sem, val, key = ops[d].event
                            if known.get(key, 0) < val and need.get(key, (None, 0))[1] < val:
                                need[key] = (sem, val)
                        if o.dma:
                            sem, val, key = o.event
                            if val > 16 and known.get(key, 0) < val - 16 and need.get(key, (None, 0))[1] < val - 16:
                                need[key] = (sem, val - 16)
                        for key, (sem, val) in need.items():
                            e.wait_ge(sem, val)
                            known[key] = val
                        mname, ma, mk_ = o.fn
                        ins = getattr(e, mname)(*ma, **mk_)
                        if o.event is not None:
                            ins.then_inc(o.event[0], 16 if o.dma else 1)
                    if ename in dsem:
                        for k, s in enumerate(dsem[ename]):
                            if dcnt[ename][k] > 0 and known.get(("d", ename, k), 0) < dcnt[ename][k]:
                                e.wait_ge(s, dcnt[ename][k])
                getattr(block, engmap[ename])(body)


class Region:
    def __init__(self, nc, stack, name, nbytes, cell=512):
        self.t = stack.enter_context(nc.sbuf_tensor(name, [128, nbytes], U8))
        self.cell = cell
        self.n = nbytes
        self.cells = [Res() for _ in range((nbytes + cell - 1) // cell)]
        self.top = 0

    def view(self, off, shape, dtype):
        esz = {F32: 4, BF16: 2, I32: 4, U8: 1}[dtype]
        n = int(np.prod(shape)) * esz
        assert off % 4 == 0 and off + n <= self.n, (off, n, self.n)
        ap = self.t[:, off:off + n]
        if dtype != U8:
            ap = ap.bitcast(dtype)
        if len(shape) > 1:
            names = [f"d{i}" for i in range(len(shape))]
            kw = {names[i]: int(shape[i]) for i in range(1, len(shape))}
            ap = ap.rearrange(f"p ({' '.join(names)}) -> p {' '.join(names)}", **kw)
        return Buf(ap, self.cells[off // self.cell:(off + n - 1) // self.cell + 1])

    def alloc(self, shape, dtype):
        esz = {F32: 4, BF16: 2, I32: 4, U8: 1}[dtype]
        n = int(np.prod(shape)) * esz
        off = (self.top + self.cell - 1) // self.cell * self.cell
        self.top = off + n
        return self.view(off, shape, dtype)


def dram_buf(ap):
    return Buf(ap, [Res()])


def strips_from_cols(W, colidx, mc):
    K = W.shape[0]
    kt = K // 128
    colidx = np.asarray(colidx)
    ns = len(colidx) // mc
    Wp = np.concatenate([W, np.zeros((K, 1), W.dtype)], axis=1)
    G = Wp[:, colidx]
    G = G.reshape(kt, 128, ns, mc).transpose(2, 1, 0, 3)
    return np.ascontiguousarray(G.reshape(ns, 128, kt * mc))


def per_part(v, ntile):
    return np.ascontiguousarray(np.asarray(v, np.float32).reshape(ntile, 128).T)


def win_plan():
    rq0, rk0, rv0, rg0, mqk0, mv0, mo0, mg0, su0 = 0, 384, 768, 1536, 2304, 3840, 4608, 5376, 5388

    def plain(base, i):
        return list(range(base + i * 128, base + (i + 1) * 128))

    def swap(base, i):
        idx = []
        for h in (2 * i, 2 * i + 1):
            idx += list(range(base + h * 64 + 32, base + h * 64 + 64)) + list(range(base + h * 64, base + h * 64 + 32))
        return idx
    fmr = []
    for i in range(3):
        fmr += plain(rq0, i) + swap(rq0, i)
    for i in range(3):
        fmr += plain(rk0, i) + swap(rk0, i)
    fmm = list(range(mqk0, mqk0 + 1536))
    fms = list(range(su0, su0 + 512))
    tm_ret = list(range(rv0, rv0 + 768)) + list(range(rg0, rg0 + 768))
    tm_ml = list(range(mv0, mv0 + 768)) + list(range(mo0, mo0 + 768))
    gates = list(range(mg0, mg0 + 12)) + [-1] * 4
    return fmr, fmm, fms, tm_ret, tm_ml, gates


class Cfg:
    def __init__(self, seq=SEQ, depth=DEPTH, mixer=True, ffn=True, parts=("ret", "ml", "s5")):
        self.seq, self.depth, self.mixer, self.ffn, self.parts = seq, depth, mixer, ffn, tuple(parts)
        self.ntile = seq // TT


WKINDS = (("fmr", 6, 4096), ("tmr", 6, 4096), ("fmm", 6, 4096), ("gt", 1, 256), ("tmm", 6, 4096),
          ("fms", 2, 4096), ("s5w", 16, 512), ("glu", 1, 2048), ("out", 8, 4096), ("up", 44, 4096), ("dn", 32, 2816))
MIXK = ("fmr", "tmr", "fmm", "tmm", "gt", "fms", "s5w", "glu", "out")
KSCALE = 128.0 ** -0.5
TWO_PI = 2.0 * math.pi


def cs(c):
    return slice(c * 128, (c + 1) * 128)


def build(cfg):
    nc = bass.Bass("TRN2", target_bir_lowering=False)
    din = {}

    def inp(name, shape, dt=F32):
        din[name] = nc.dram_tensor(name, list(shape), dt, kind="ExternalInput").ap()
        return din[name]

    def kind_on(kind):
        if kind in ("up", "dn"):
            return cfg.ffn
        if not cfg.mixer:
            return False
        if kind in ("fmr", "tmr"):
            return "ret" in cfg.parts
        if kind in ("fmm", "tmm", "gt"):
            return "ml" in cfg.parts
        if kind in ("fms", "s5w", "glu"):
            return "s5" in cfg.parts and not getattr(cfg, "s5_skip_main", False)
        return True

    x_d = inp("x", [cfg.seq, D])
    out_d = nc.dram_tensor("out", [cfg.seq, D], F32, kind="ExternalOutput").ap()
    w32 = {}
    w16 = {}
    for l in range(cfg.depth):
        for kind, ns, ne in WKINDS:
            w32[l, kind] = inp(f"w_{kind}_{l}", [ns, 128, ne])
            w16[l, kind] = nc.dram_tensor(f"s_{kind}_{l}", [ns, 128, ne], BF16, kind="Internal").ap()
    nw_d = inp("nw", [128, 2 * cfg.depth + 1, KT])
    fcw_d = inp("fcw", [128, cfg.depth, 88, 3])
    fcb_d = inp("fcb", [128, cfg.depth, 88])
    ident_d = inp("ident", [128, 128])
    ropec_d = inp("ropec", [128, cfg.seq])
    ropes_d = inp("ropes", [128, cfg.seq])
    rtab_d = inp("rtab", [128, 12, 128])
    tri_d = inp("tri", [128, 128])
    mcw_d = inp("mcw", [128, cfg.depth, 12, 4])
    mcb_d = inp("mcb", [128, cfg.depth, 12])
    gbias_d = inp("gbias", [cfg.depth, 12])
    gnw_d = inp("gnw", [128, cfg.depth, 12])
    s5a_d = inp("s5a", [128, cfg.depth, 3, 16])
    s5d_d = inp("s5d", [128, cfg.depth, 4])
    glub_d = inp("glub", [128, cfg.depth, 4])
    jidx_d = inp("jidx", [128, 516])
    tab_d = nc.dram_tensor("s5tab", [cfg.depth, 16, 128, 2048], F32, kind="Internal").ap()
    GAM = [1.0 - 2.0 ** (-5.0 - h) for h in range(6)]
    GL = [float(np.exp(np.float32(128.0) * np.log1p(-np.exp2(np.float32(-5.0 - h))))) for h in range(6)]

    with ExitStack() as st0:
        P0 = Prog()
        R0 = Region(nc, st0, "r0", 4 * 16384 + 4 * 8192, cell=8192)
        sin = [R0.alloc([4096], F32) for _ in range(4)]
        sout = [R0.alloc([4096], BF16) for _ in range(4)]
        jobs = []
        for l in range(cfg.depth):
            for kind, ns, ne in WKINDS:
                if not kind_on(kind):
                    continue
                for s in range(ns):
                    jobs.append((w32[l, kind][s], w16[l, kind][s], ne))
        n = len(jobs)
        LOOK = 3

        def p0_load(i):
            src, dst, ne = jobs[i]
            a = sin[i % 4]
            P0.op("sp", lambda e: e.dma_start(out=a.ap[:, 0:ne], in_=src), writes=[a], dma=True)

        for i in range(min(LOOK, n)):
            p0_load(i)
        for i in range(n):
            src, dst, ne = jobs[i]
            a, b = sin[i % 4], sout[i % 4]
            if i % 2 == 0:
                P0.op("act", lambda e: e.copy(out=b.ap[:, 0:ne], in_=a.ap[:, 0:ne]), reads=[a], writes=[b])
            else:
                P0.op("dve", lambda e: e.tensor_copy(out=b.ap[:, 0:ne], in_=a.ap[:, 0:ne]), reads=[a], writes=[b])
            P0.op("pool", lambda e: e.dma_start(out=dst, in_=b.ap[:, 0:ne]), reads=[b], dma=True)
            if i + LOOK < n:
                p0_load(i + LOOK)
        if n:
            P0.emit(nc, st0)

    with ExitStack() as st:
        P = Prog()
        RP = Region(nc, st, "rp", 126 * 1024, cell=256)
        RA = Region(nc, st, "ra", 44 * 1024, cell=512)
        RB = Region(nc, st, "rb", 36 * 1024, cell=256)
        xT = [RP.alloc([TT], F32) for _ in range(KT)]
        hT = [RP.alloc([TT], BF16) for _ in range(KT)]
        NW = 3
        wb = [RP.alloc([4096], BF16) for _ in range(NW)]
        ident_f = RP.alloc([128], F32)
        ident_b = RP.alloc([128], BF16)
        ones_b = RP.alloc([128], BF16)
        ones_f = RP.alloc([128], F32)
        tri_f = RP.alloc([128], F32)
        nw = RP.alloc([2 * cfg.depth + 1, KT], F32)
        fcw = RP.alloc([cfg.depth, 88, 3], F32)
        fcb = RP.alloc([cfg.depth, 88], F32)
        fhist = RP.alloc([cfg.depth, 88, 2], BF16)
        rtab = RP.alloc([12, 128], F32)
        mcw = RP.alloc([cfg.depth, 12, 4], F32)
        mcb = RP.alloc([cfg.depth, 12], F32)
        mhist = RP.alloc([cfg.depth, 12, 4], BF16)
        gbias = RP.alloc([cfg.depth, 12], F32)
        gnw = RP.alloc([cfg.depth, 12], F32)
        nhalf = RP.alloc([1], F32)
        rstf = [[RP.alloc([128], F32) for h in range(6)] for l in range(cfg.depth)]
        rstb = [[RP.alloc([128], BF16) for h in range(6)] for l in range(cfg.depth)]
        mstf = [[RP.alloc([132], F32) for h in range(6)] for l in range(cfg.depth)]
        mmst = [RP.alloc([6], F32) for l in range(cfg.depth)]
        s5c = RP.alloc([cfg.depth, 8, 16], F32)
        s5z = RP.alloc([cfg.depth, 2, 16], F32)
        s5d = RP.alloc([cfg.depth, 4], F32)
        glub = RP.alloc([cfg.depth, 4], F32)
        arena = [RA.alloc([TT], BF16) for _ in range(44)]
        psb = [Buf(st.enter_context(nc.psum_tensor(f"ps{i}", [128, 512], F32))[:], [Res(excl=True)]) for i in range(8)]
        pstate = {"n": 0, "pool": list(range(8)), "ln": 0, "lpool": None}

        def ps_next(long=False):
            if long and pstate["lpool"]:
                lp = pstate["lpool"]
                b = psb[lp[pstate["ln"] % len(lp)]]
                pstate["ln"] += 1
                return b
            pool = pstate["pool"]
            b = psb[pool[pstate["n"] % len(pool)]]
            pstate["n"] += 1
            return b

        def ps_split(on):
            if on:
                pstate["pool"], pstate["lpool"] = [3, 4, 5, 6, 7], [0, 1, 2]
            else:
                pstate["pool"], pstate["lpool"] = list(range(8)), None

        def dma_in(dst, src, q="sp"):
            P.op(q, lambda e: e.dma_start(out=dst.ap, in_=src), writes=[dst], dma=True)

        dma_in(ident_f, ident_d)
        dma_in(nw, nw_d)
        dma_in(fcw, fcw_d)
        dma_in(fcb, fcb_d)
        dma_in(rtab, rtab_d)
        dma_in(tri_f, tri_d)
        dma_in(mcw, mcw_d)
        dma_in(mcb, mcb_d)
        dma_in(gnw, gnw_d)
        dma_in(s5d, s5d_d)
        dma_in(glub, glub_d)
        for l in range(cfg.depth):
            P.op("sp", lambda e, l=l: e.dma_start(out=gbias.ap[:, l, :], in_=gbias_d[l].partition_broadcast(128)),
                 writes=[gbias], dma=True)
        P.op("dve", lambda e: e.tensor_copy(out=ident_b.ap, in_=ident_f.ap), reads=[ident_f], writes=[ident_b])
        P.op("dve", lambda e: e.memset(ones_b.ap, 1.0), writes=[ones_b])
        P.op("dve", lambda e: e.memset(ones_f.ap, 1.0), writes=[ones_f])
        P.op("dve", lambda e: e.memset(nhalf.ap, -0.5), writes=[nhalf])
        P.op("dve", lambda e: e.memset(fhist.ap, 0.0), writes=[fhist])
        P.op("dve", lambda e: e.memset(mhist.ap, 0.0), writes=[mhist])
        P.op("dve", lambda e: e.memset(s5z.ap, 0.0), writes=[s5z])
        for l in range(cfg.depth):
            P.op("dve", lambda e, l=l: e.memset(mmst[l].ap, 0.0), writes=[mmst[l]])
            for h in range(6):
                P.op("pool", lambda e, b=rstf[l][h]: e.memset(b.ap, 0.0), writes=[rstf[l][h]])
                P.op("pool", lambda e, b=rstb[l][h]: e.memset(b.ap, 0.0), writes=[rstb[l][h]])
                P.op("pool", lambda e, b=mstf[l][h]: e.memset(b.ap, 0.0), writes=[mstf[l][h]])

        tabres = [[dram_buf(tab_d[l, t]) for t in range(16)] for l in range(cfg.depth)]

        def cossin(ang, width, ki, kf, sh, ch, co, si):
            P.op("dve", lambda e: e.tensor_scalar(out=ki, in0=ang, scalar1=1.0 / TWO_PI, scalar2=None, op0=ALU.mult),
                 reads=[SB], writes=[SB])
            P.op("dve", lambda e: e.tensor_copy(out=kf, in_=ki), reads=[SB], writes=[SB])
            P.op("dve", lambda e: e.scalar_tensor_tensor(out=kf, in0=kf, scalar=-TWO_PI, in1=ang, op0=ALU.mult,
                                                         op1=ALU.add), reads=[SB], writes=[SB])
            P.op("act", lambda e: e.activation(out=sh, in_=kf, func=AF.Sin, scale=0.5), reads=[SB], writes=[SB])
            P.op("act", lambda e: e.activation(out=kf, in_=kf, func=AF.Abs), reads=[SB], writes=[SB])
            P.op("act", lambda e: e.activation(out=ch, in_=kf, func=AF.Sin, scale=-0.5, bias=math.pi / 2),
                 reads=[SB], writes=[SB])
            P.op("dve", lambda e: e.tensor_tensor(out=co, in0=sh, in1=sh, op=ALU.mult), reads=[SB], writes=[SB])
            P.op("dve", lambda e: e.tensor_scalar(out=co, in0=co, scalar1=-2.0, scalar2=1.0, op0=ALU.mult, op1=ALU.add),
                 reads=[SB], writes=[SB])
            P.op("dve", lambda e: e.scalar_tensor_tensor(out=si, in0=sh, scalar=2.0, in1=ch, op0=ALU.mult,
                                                         op1=ALU.mult), reads=[SB], writes=[SB])

        if cfg.mixer and "s5" in cfg.parts:
            SB = RB.view(0, [7, 520], F32)
            SBi = RB.view(0, [7, 520], I32)
            s5a = RB.view(16384, [cfg.depth, 3, 16], F32)
            s5f = RB.view(17408, [2048], F32)
            jidx = RB.view(33792, [516], F32)
            tst = [RB.view(25600, [4, 512], F32)]
            dma_in(s5a, s5a_d)
            dma_in(jidx, jidx_d)
            for l in range(cfg.depth):
                ar, ai, ls = s5a.ap[:, l, 0, :], s5a.ap[:, l, 1, :], s5a.ap[:, l, 2, :]
                v = lambda i, w=16: SB.ap[:, i, 0:w]
                vi = lambda i, w=16: SBi.ap[:, i, 0:w]
                C = lambda i: s5c.ap[:, l, i, :]
                RW = dict(reads=[SB, s5a, s5c], writes=[SB, s5c])
                P.op("act", lambda e: e.activation(out=v(0), in_=ls, func=AF.Exp), **RW)
                P.op("dve", lambda e: e.tensor_tensor(out=v(1), in0=ar, in1=v(0), op=ALU.mult), **RW)
                P.op("act", lambda e: e.activation(out=C(0), in_=v(1), func=AF.Exp), **RW)
                P.op("dve", lambda e: e.tensor_tensor(out=v(1), in0=ai, in1=v(0), op=ALU.mult), **RW)
                cossin(v(1), 16, vi(2), v(2), v(3), v(4), v(5), v(6))
                P.op("dve", lambda e: e.tensor_scalar(out=vi(2), in0=v(1), scalar1=1.0 / TWO_PI, scalar2=None,
                                                      op0=ALU.mult), **RW)
                P.op("dve", lambda e: e.tensor_copy(out=v(3), in_=vi(2)), **RW)
                P.op("dve", lambda e: e.scalar_tensor_tensor(out=C(7), in0=v(3), scalar=-TWO_PI, in1=v(1),
                                                             op0=ALU.mult, op1=ALU.add), **RW)
                P.op("dve", lambda e: e.tensor_tensor(out=v(0), in0=C(0), in1=v(5), op=ALU.mult), **RW)
                P.op("dve", lambda e: e.tensor_scalar(out=v(0), in0=v(0), scalar1=-1.0, scalar2=None, op0=ALU.add), **RW)
                P.op("dve", lambda e: e.tensor_tensor(out=v(1), in0=C(0), in1=v(6), op=ALU.mult), **RW)
                P.op("dve", lambda e: e.tensor_tensor(out=v(2), in0=ar, in1=ar, op=ALU.mult), **RW)
                P.op("dve", lambda e: e.tensor_tensor(out=v(3), in0=ai, in1=ai, op=ALU.mult), **RW)
                P.op("dve", lambda e: e.tensor_tensor(out=v(2), in0=v(2), in1=v(3), op=ALU.add), **RW)
                P.op("dve", lambda e: e.reciprocal(out=v(2), in_=v(2)), **RW)
                P.op("dve", lambda e: e.tensor_tensor(out=v(3), in0=v(0), in1=ar, op=ALU.mult), **RW)
                P.op("dve", lambda e: e.tensor_tensor(out=v(4), in0=v(1), in1=ai, op=ALU.mult), **RW)
                P.op("dve", lambda e: e.tensor_tensor(out=v(3), in0=v(3), in1=v(4), op=ALU.add), **RW)
                P.op("dve", lambda e: e.tensor_tensor(out=C(1), in0=v(3), in1=v(2), op=ALU.mult), **RW)
                P.op("dve", lambda e: e.tensor_tensor(out=v(3), in0=v(1), in1=ar, op=ALU.mult), **RW)
                P.op("dve", lambda e: e.tensor_tensor(out=v(4), in0=v(0), in1=ai, op=ALU.mult), **RW)
                P.op("dve", lambda e: e.tensor_tensor(out=v(3), in0=v(3), in1=v(4), op=ALU.subtract), **RW)
                P.op("dve", lambda e: e.tensor_tensor(out=C(2), in0=v(3), in1=v(2), op=ALU.mult), **RW)
                P.op("dve", lambda e: e.tensor_scalar(out=C(3), in0=C(1), scalar1=-1.0, scalar2=None, op0=ALU.mult), **RW)
                for t in range(16):
                    W = 513
                    RW2 = dict(reads=[SB, s5c, jidx], writes=[SB])
                    P.op("dve", lambda e, t=t: e.tensor_scalar(out=v(0, W), in0=jidx.ap[:, 0:W],
                                                               scalar1=s5c.ap[:, l, 7, t:t + 1], scalar2=None,
                                                               op0=ALU.mult), **RW2)
                    cossin(v(0, W), W, vi(1, W), v(1, W), v(2, W), v(3, W), v(4, W), v(5, W))
                    tb = tst[0]
                    RW3 = dict(reads=[SB, s5c], writes=[SB, tb, s5c])
                    P.op("dve", lambda e, t=t: e.tensor_scalar(out=v(1, 512), in0=v(4, 512),
                                                               scalar1=s5c.ap[:, l, 1, t:t + 1], scalar2=None,
                                                               op0=ALU.mult), **RW3)
                    P.op("dve", lambda e, t=t, tb=tb: e.scalar_tensor_tensor(
                        out=tb.ap[:, 0, :], in0=v(5, 512), scalar=s5c.ap[:, l, 2, t:t + 1], in1=v(1, 512),
                        op0=ALU.mult, op1=ALU.add), **RW3)
                    P.op("dve", lambda e, t=t: e.tensor_scalar(out=v(1, 512), in0=v(4, 512),
                                                               scalar1=s5c.ap[:, l, 2, t:t + 1], scalar2=None,
                                                               op0=ALU.mult), **RW3)
                    P.op("dve", lambda e, t=t, tb=tb: e.scalar_tensor_tensor(
                        out=tb.ap[:, 1, :], in0=v(5, 512), scalar=s5c.ap[:, l, 3, t:t + 1], in1=v(1, 512),
                        op0=ALU.mult, op1=ALU.add), **RW3)
                    P.op("act", lambda e, tb=tb: e.copy(out=tb.ap[:, 2, :], in_=v(4, 512)), **RW3)
                    P.op("act", lambda e, tb=tb: e.copy(out=tb.ap[:, 3, :], in_=v(5, 512)), **RW3)
                    P.op("dve", lambda e, t=t: e.tensor_copy(out=s5c.ap[:, l, 4, t:t + 1], in_=v(4, W)[:, 512:513]), **RW3)
                    P.op("dve", lambda e, t=t: e.tensor_copy(out=s5c.ap[:, l, 5, t:t + 1], in_=v(5, W)[:, 512:513]), **RW3)
                    P.op("dve", lambda e, t=t: e.tensor_scalar(out=s5c.ap[:, l, 6, t:t + 1], in0=v(5, W)[:, 512:513],
                                                               scalar1=-1.0, scalar2=None, op0=ALU.mult), **RW3)
                    P.op("sp", lambda e, t=t, tb=tb: e.dma_start(
                        out=tab_d[l, t], in_=tb.ap.rearrange("p a b -> p (a b)")), reads=[tb], writes=[tabres[l][t]], dma=True)

        strip_seq = []
        for t in range(cfg.ntile):
            for l in range(cfg.depth):
                for kind, ns, ne in WKINDS:
                    if not kind_on(kind):
                        continue
                    for s in range(ns):
                        strip_seq.append((l, kind, s, ne))
        wstate = {"issued": 0, "used": 0}

        def w_issue():
            i = wstate["issued"]
            if i >= len(strip_seq):
                return
            l, kind, s, ne = strip_seq[i]
            slot = wb[i % NW]
            src = w16[l, kind][s]
            P.op("sp", lambda e, slot=slot, src=src, ne=ne: e.dma_start(out=slot.ap[:, 0:ne], in_=src),
                 writes=[slot], dma=True)
            wstate["issued"] += 1

        def w_next(l, kind, s):
            i = wstate["used"]
            assert strip_seq[i][:3] == (l, kind, s), (strip_seq[i], l, kind, s)
            while wstate["issued"] < min(i + NW, len(strip_seq)):
                w_issue()
            wstate["used"] += 1
            return wb[i % NW]

        def rmsnorm_stats(sqb, rstd):
            ps = ps_next()
            for kt in range(KT):
                sq = sqb[kt % 2]
                P.op("act", lambda e, sq=sq, kt=kt: e.activation(out=sq.ap, in_=xT[kt].ap, func=AF.Square),
                     reads=[xT[kt]], writes=[sq])
                P.op("pe", lambda e, sq=sq, kt=kt, ps=ps: e.matmul(ps.ap, lhsT=ones_b.ap, rhs=sq.ap,
                                                                 start=(kt == 0), stop=(kt == KT - 1)),
                     reads=[sq, ones_b], writes=[ps])
            P.op("act", lambda e, ps=ps: e.activation(out=rstd.ap, in_=ps.ap, func=AF.Sqrt, bias=EPS, scale=1.0 / D),
                 reads=[ps], writes=[rstd])
            P.op("dve", lambda e: e.reciprocal(out=rstd.ap, in_=rstd.ap), reads=[rstd], writes=[rstd])

        def apply_norm(nidx, rstd):
            for kt in range(KT):
                P.op("dve", lambda e, kt=kt: e.scalar_tensor_tensor(
                    out=hT[kt].ap, in0=xT[kt].ap, scalar=nw.ap[:, nidx, kt:kt + 1], in1=rstd.ap,
                    op0=ALU.mult, op1=ALU.mult), reads=[xT[kt], nw, rstd], writes=[hT[kt]])

        def fm_tile(w, half, ps):
            wv = w.ap.rearrange("p (k c) -> p k c", c=256)
            for kt in range(KT):
                P.op("pe", lambda e, kt=kt: e.matmul(ps.ap, lhsT=wv[:, kt, half * 128:(half + 1) * 128], rhs=hT[kt].ap,
                                                     start=(kt == 0), stop=(kt == KT - 1)),
                     reads=[w, hT[kt]], writes=[ps])

        def tm_strip(w, evac):
            wv = w.ap.rearrange("p (k c) -> p k c", c=256)
            for cp in range(2):
                ps = ps_next()
                for ci in range(2):
                    c = cp * 2 + ci
                    for kt in range(KT):
                        P.op("pe", lambda e, kt=kt, c=c, ci=ci, ps=ps: e.matmul(
                            ps.ap[:, ci * 256:(ci + 1) * 256], lhsT=hT[kt].ap[:, cs(c)], rhs=wv[:, kt, :],
                            start=(kt == 0), stop=(kt == KT - 1)), reads=[w, hT[kt]], writes=[ps])
                evac(ps, cp)

        ropec = RB.view(0, [TT], F32)
        ropes = RB.view(2048, [TT], F32)
        tA = [RB.view(4096 + i * 2048, [TT], F32) for i in range(2)]
        tB = [RB.view(8192 + i * 2048, [TT], F32) for i in range(2)]
        pcb = [RB.view(4096 + i * 1032, [TT + 4], BF16) for i in range(2)]
        mdiag = [[RB.view(8192 + (i * 4 + k) * 256, [128], BF16) for k in range(4)] for i in range(2)]
        spt = [RB.view(12288 + i * 256, [128], BF16) for i in range(2)]
        kd = [RB.view(12800 + i * 256, [128], BF16) for i in range(2)]
        t1 = [RB.view(13312 + i * 512, [128], F32) for i in range(2)]
        gG = [RB.view(14336 + i * 256, [128], BF16) for i in range(2)]
        stb = [RB.view(14848 + i * 264, [132], BF16) for i in range(2)]
        small2 = [[RB.view(2048 + (p_ * 4 + i) * 256, [16], F32) for i in range(4)] for p_ in range(2)]
        small = small2[0]
        gsm = RB.view(16384, [12, 24], F32)
        xg = RB.view(17664, [128], F32)
        mixT = [RB.view(20480 + i * 1024, [TT], BF16) for i in range(KT)]
        rq_rot, rq_dec, rk_rot, kpad = arena[0:3], arena[3:6], arena[6:9], arena[9:15]
        mq, mk = arena[15:21], arena[21:27]
        Vc = [None] * NCH
        for c in range(NCH):
            off = 27 * 1024 + c * 1536
            Vc[c] = RA.view(off, [6, 128], BF16)
        Gc = [RA.view(33 * 1024 + c * 1536, [6, 128], BF16) for c in range(NCH)]

        postn = [0]

        def post(l, ps, c, tile, gate_ap, den=None):
            postn[0] += 1
            k = postn[0] % 2
            sset = small2[postn[0] % 2]
            st6, mv, rs = sset[0], sset[1], sset[2]
            P.op("dve", lambda e: e.bn_stats(out=st6.ap[:, 0:6], in_=ps.ap[:, 0:128]), reads=[ps], writes=[st6])
            P.op("dve", lambda e: e.bn_aggr(out=mv.ap[:, 0:2], in_=st6.ap[:, 0:6]), reads=[st6], writes=[mv])
            if den is None:
                P.op("dve", lambda e: e.tensor_scalar(out=rs.ap[:, 0:1], in0=mv.ap[:, 1:2], scalar1=EPS, scalar2=None,
                                                      op0=ALU.add), reads=[mv], writes=[rs])
            else:
                P.op("dve", lambda e: e.tensor_tensor(out=rs.ap[:, 1:2], in0=den.ap[:, 0:1], in1=den.ap[:, 0:1],
                                                      op=ALU.mult), reads=[den], writes=[rs])
                P.op("dve", lambda e: e.scalar_tensor_tensor(out=rs.ap[:, 0:1], in0=rs.ap[:, 1:2], scalar=EPS,
                                                             in1=mv.ap[:, 1:2], op0=ALU.mult, op1=ALU.add),
                     reads=[rs, mv], writes=[rs])
            P.op("act", lambda e: e.activation(out=rs.ap[:, 3:4], in_=rs.ap[:, 0:1], func=AF.Sqrt), reads=[rs], writes=[rs])
            P.op("dve", lambda e: e.reciprocal(out=rs.ap[:, 2:3], in_=rs.ap[:, 3:4]), reads=[rs], writes=[rs])
            tt = t1[k]
            P.op("dve", lambda e: e.scalar_tensor_tensor(out=tt.ap, in0=ps.ap[:, 0:128], scalar=mv.ap[:, 0:1],
                                                         in1=gate_ap[1], op0=ALU.subtract, op1=ALU.mult),
                 reads=[ps, mv, gate_ap[0]], writes=[tt])
            g = gG[k]
            P.op("pool", lambda e: e.tensor_scalar(out=g.ap, in0=tt.ap, scalar1=rs.ap[:, 2:3], scalar2=1.0,
                                                   op0=ALU.mult, op1=ALU.mult), reads=[tt, rs], writes=[g])
            ps2 = ps_next()
            pv = ps2.ap.bitcast(BF16)
            P.op("pe", lambda e: e.transpose(pv[:, 0:128], g.ap, ident_b.ap), reads=[g, ident_b], writes=[ps2])
            P.op("act", lambda e: e.activation(out=mixT[tile].ap[:, cs(c)], in_=pv[:, 0:128], func=AF.Identity,
                                               scale=gnw.ap[:, l, tile:tile + 1]),
                 reads=[ps2, gnw], writes=[mixT[tile]])

        def retention(l, t, inter=None):
            P.op("pool", lambda e: e.dma_start(out=ropec.ap, in_=ropec_d[:, t * TT:(t + 1) * TT]), writes=[ropec], dma=True)
            P.op("pool", lambda e: e.dma_start(out=ropes.ap, in_=ropes_d[:, t * TT:(t + 1) * TT]), writes=[ropes], dma=True)
            for h in range(6):
                half = h % 2
                P.op("pool", lambda e, h=h, half=half: e.memset(kpad[h].ap[(1 - half) * 64:(2 - half) * 64, :], 0.0),
                     writes=[kpad[h]])
            for s in range(6):
                w = w_next(l, "fmr", s)
                psx, pss = ps_next(), ps_next()
                fm_tile(w, 0, psx)
                fm_tile(w, 1, pss)
                isq = s < 3
                i = s % 3
                a, b = tA[s % 2], tB[s % 2]
                sc = 0.125 if isq else 1.0
                P.op("dve", lambda e, a=a, psx=psx, sc=sc: e.scalar_tensor_tensor(
                    out=a.ap, in0=psx.ap, scalar=sc, in1=ropec.ap, op0=ALU.mult, op1=ALU.mult),
                    reads=[psx, ropec], writes=[a])
                P.op("dve", lambda e, b=b, pss=pss, sc=sc: e.scalar_tensor_tensor(
                    out=b.ap, in0=pss.ap, scalar=sc, in1=ropes.ap, op0=ALU.mult, op1=ALU.mult),
                    reads=[pss, ropes], writes=[b])
                dst = rq_rot[i] if isq else rk_rot[i]
                P.op("pool", lambda e, a=a, b=b, dst=dst: e.tensor_tensor(out=dst.ap, in0=a.ap, in1=b.ap, op=ALU.add),
                     reads=[a, b], writes=[dst])
                if isq:
                    for c in range(NCH):
                        P.op("pool", lambda e, c=c, i=i, dst=dst: e.tensor_tensor(
                            out=rq_dec[i].ap[:, cs(c)], in0=dst.ap[:, cs(c)], in1=rtab.ap[:, 6 + i, :], op=ALU.mult),
                            reads=[dst, rtab], writes=[rq_dec[i]])
                else:
                    for half in range(2):
                        P.op("pool", lambda e, i=i, half=half, dst=dst: e.tensor_copy(
                            out=kpad[2 * i + half].ap[half * 64:(half + 1) * 64, :],
                            in_=dst.ap[half * 64:(half + 1) * 64, :]), reads=[dst], writes=[kpad[2 * i + half]])
            for s in range(6):
                w = w_next(l, "tmr", s)
                isv = s < 3
                hh = (s % 3) * 2

                def evac(ps, cp, isv=isv, hh=hh):
                    for ci in range(2):
                        c = cp * 2 + ci
                        src = ps.ap[:, ci * 256:(ci + 1) * 256].rearrange("p (h v) -> p h v", v=128)
                        if isv:
                            P.op("dve", lambda e, c=c, src=src: e.tensor_copy(out=Vc[c].ap[:, hh:hh + 2, :], in_=src),
                                 reads=[ps], writes=[Vc[c]])
                        else:
                            P.op("act", lambda e, c=c, src=src: e.activation(out=Gc[c].ap[:, hh:hh + 2, :], in_=src,
                                                                             func=AF.Silu),
                                 reads=[ps], writes=[Gc[c]])
                tm_strip(w, evac)
            ps_split(True)
            for c in range(NCH):
                kds = []
                for i in range(3):
                    ps = ps_next()
                    pv = ps.ap.bitcast(BF16)
                    P.op("pe", lambda e, i=i, pv=pv: e.transpose(pv[:, 0:128], rk_rot[i].ap[:, cs(c)], ident_b.ap),
                         reads=[rk_rot[i], ident_b], writes=[ps])
                    kdi = RB.view(18432 + i * 256, [128], BF16)
                    P.op("dve", lambda e, i=i, pv=pv, kdi=kdi: e.tensor_tensor(
                        out=kdi.ap, in0=pv[:, 0:128], in1=rtab.ap[:, 9 + i, :], op=ALU.mult),
                        reads=[ps, rtab], writes=[kdi])
                    kds.append(kdi)
                for h in range(6):
                    i, half = h // 2, h % 2
                    k = h % 2
                    ps = ps_next()
                    P.op("pe", lambda e, h=h, i=i, ps=ps: e.matmul(ps.ap[:, 0:128], lhsT=kpad[h].ap[:, cs(c)],
                                                                 rhs=rq_rot[i].ap[:, cs(c)], start=True, stop=True),
                         reads=[kpad[h], rq_rot[i]], writes=[ps])
                    sp = spt[k]
                    P.op("dve", lambda e, h=h, ps=ps, sp=sp: e.tensor_tensor(out=sp.ap, in0=ps.ap[:, 0:128],
                                                                           in1=rtab.ap[:, h, :], op=ALU.mult),
                         reads=[ps, rtab], writes=[sp])
                    po = ps_next(long=True)
                    P.op("pe", lambda e, h=h, po=po, sp=sp: e.matmul(po.ap[:, 0:128], lhsT=sp.ap, rhs=Vc[c].ap[:, h, :],
                                                                   start=True, stop=False),
                         reads=[sp, Vc[c]], writes=[po])
                    P.op("pe", lambda e, h=h, i=i, po=po: e.matmul(po.ap[:, 0:128], lhsT=rq_dec[i].ap[:, cs(c)],
                                                                 rhs=rstb[l][h].ap, start=False, stop=True),
                         reads=[rq_dec[i], rstb[l][h]], writes=[po])
                    pk = ps_next()
                    P.op("pe", lambda e, h=h, i=i, pk=pk: e.matmul(pk.ap[:, 0:128], lhsT=kds[i].ap, rhs=Vc[c].ap[:, h, :],
                                                                 start=True, stop=True),
                         reads=[kds[i], Vc[c]], writes=[pk])
                    rows = slice(half * 64, (half + 1) * 64)
                    P.op("dve", lambda e, h=h, pk=pk, rows=rows: e.scalar_tensor_tensor(
                        out=rstf[l][h].ap[rows, :], in0=rstf[l][h].ap[rows, :], scalar=GL[h], in1=pk.ap[rows, 0:128],
                        op0=ALU.mult, op1=ALU.add), reads=[pk, rstf[l][h]], writes=[rstf[l][h]])
                    P.op("pool", lambda e, h=h, rows=rows: e.tensor_copy(out=rstb[l][h].ap[rows, :],
                                                                        in_=rstf[l][h].ap[rows, :]),
                         reads=[rstf[l][h]], writes=[rstb[l][h]])
                    post(l, po, c, h, (Gc[c], Gc[c].ap[:, h, :]))
                if inter is not None:
                    inter(c)
            ps_split(False)

        def mlstm_fm(l, s):
            if True:
                w = w_next(l, "fmm", s)
                for half in range(2):
                    tile = s * 2 + half
                    ps = ps_next()
                    fm_tile(w, half, ps)
                    pc = pcb[tile % 2]
                    dg = mdiag[tile % 2]
                    P.op("pool", lambda e, pc=pc, tile=tile: e.tensor_copy(out=pc.ap[:, 0:4], in_=mhist.ap[:, l, tile, :]),
                         reads=[mhist], writes=[pc])
                    P.op("act", lambda e, pc=pc, ps=ps: e.copy(out=pc.ap[:, 4:4 + TT], in_=ps.ap), reads=[ps], writes=[pc])
                    P.op("pool", lambda e, pc=pc, tile=tile: e.tensor_copy(out=mhist.ap[:, l, tile, :],
                                                                         in_=pc.ap[:, TT:TT + 4]),
                         reads=[pc], writes=[mhist])
                    for k in range(4):
                        if k % 2 == 0:
                            P.op("dve", lambda e, d=dg[k], tile=tile, k=k: e.tensor_scalar(
                                out=d.ap, in0=ident_b.ap, scalar1=mcw.ap[:, l, tile, k:k + 1], scalar2=None, op0=ALU.mult),
                                reads=[ident_b, mcw], writes=[dg[k]])
                        else:
                            P.op("act", lambda e, d=dg[k], tile=tile, k=k: e.activation(
                                out=d.ap, in_=ident_b.ap, func=AF.Identity, scale=mcw.ap[:, l, tile, k:k + 1]),
                                reads=[ident_b, mcw], writes=[dg[k]])
                    ps2 = ps_next()
                    for k in range(4):
                        P.op("pe", lambda e, ps2=ps2, d=dg[k], pc=pc, k=k: e.matmul(
                            ps2.ap, lhsT=d.ap, rhs=pc.ap[:, k + 1:k + 1 + TT], start=(k == 0), stop=(k == 3)),
                            reads=[dg[k], pc], writes=[ps2])
                    dst = mq[tile] if tile < 6 else mk[tile - 6]
                    P.op("act", lambda e, ps2=ps2, dst=dst, tile=tile: e.activation(
                        out=dst.ap, in_=ps2.ap, func=AF.Silu, bias=mcb.ap[:, l, tile:tile + 1], scale=1.0),
                        reads=[ps2, mcb], writes=[dst])

        def mlstm(l, t, fm_done=False):
            if not fm_done:
                for s in range(6):
                    mlstm_fm(l, s)
            w = w_next(l, "gt", 0)
            wv = w.ap[:, 0:256].rearrange("p (k c) -> p k c", c=16)
            psg = ps_next()
            for c in range(NCH):
                for kt in range(KT):
                    P.op("pe", lambda e, c=c, kt=kt: e.matmul(psg.ap[:, c * 16:(c + 1) * 16], lhsT=hT[kt].ap[:, cs(c)],
                                                              rhs=wv[:, kt, :], start=(kt == 0), stop=(kt == KT - 1)),
                         reads=[w, hT[kt]], writes=[psg])
            G = lambda i: gsm.ap[:, i, :]
            G3 = lambda i: gsm.ap[:, i, :].rearrange("p (c h) -> p c h", h=6)
            RWg = dict(reads=[gsm], writes=[gsm])
            pg3 = psg.ap[:, 0:64].rearrange("p (c g) -> p c g", g=16)
            for c in range(NCH):
                P.op("dve", lambda e, c=c: e.tensor_tensor(out=G3(0)[:, c, :], in0=pg3[:, c, 0:6], in1=gbias.ap[:, l, 0:6],
                                                           op=ALU.add), reads=[psg, gbias, gsm], writes=[gsm])
                P.op("dve", lambda e, c=c: e.tensor_tensor(out=G3(1)[:, c, :], in0=pg3[:, c, 6:12], in1=gbias.ap[:, l, 6:12],
                                                           op=ALU.add), reads=[psg, gbias, gsm], writes=[gsm])
            P.op("act", lambda e: e.activation(out=G(1), in_=G(1), func=AF.Exp, scale=-1.0), **RWg)
            P.op("act", lambda e: e.activation(out=G(1), in_=G(1), func=AF.Ln, bias=1.0, scale=1.0), **RWg)
            pp = ps_next()
            for c in range(NCH):
                P.op("pe", lambda e, c=c: e.matmul(pp.ap[:, c * 6:(c + 1) * 6], lhsT=tri_f.ap, rhs=G3(1)[:, c, :],
                                                   start=True, stop=True), reads=[tri_f, gsm], writes=[pp])
                P.op("pe", lambda e, c=c: e.matmul(pp.ap[:, 32 + c * 6:32 + (c + 1) * 6], lhsT=ones_f.ap, rhs=G3(1)[:, c, :],
                                                   start=True, stop=True), reads=[ones_f, gsm], writes=[pp])
            P.op("dve", lambda e: e.tensor_tensor(out=G(2), in0=pp.ap[:, 0:24], in1=G(0), op=ALU.add),
                 reads=[pp, gsm], writes=[gsm])
            P.op("dve", lambda e: e.tensor_copy(out=G(3), in_=pp.ap[:, 0:24]), reads=[pp, gsm], writes=[gsm])
            P.op("dve", lambda e: e.tensor_copy(out=G(4), in_=pp.ap[:, 32:56]), reads=[pp, gsm], writes=[gsm])
            pt = ps_next()
            P.op("pe", lambda e: e.transpose(pt.ap[0:24, 0:128], G(2), ident_f.ap), reads=[gsm, ident_f], writes=[pt])
            P.op("dve", lambda e: e.reduce_max(out=xg.ap[0:24, 0:1], in_=pt.ap[0:24, 0:128], axis=AX.X),
                 reads=[pt], writes=[xg])
            xg2 = RB.view(18176 + 1024, [128], F32)
            P.op("dve", lambda e: e.tensor_scalar(out=xg2.ap[0:24, :], in0=ones_f.ap[0:24, :], scalar1=xg.ap[0:24, 0:1],
                                                  scalar2=None, op0=ALU.mult), reads=[xg, ones_f], writes=[xg2])
            pb = ps_next()
            P.op("pe", lambda e: e.matmul(pb.ap[:, 0:24], lhsT=xg2.ap[0:24, :], rhs=ident_f.ap[0:24, 0:24],
                                          start=True, stop=True), reads=[xg2, ident_f], writes=[pb])
            P.op("dve", lambda e: e.tensor_copy(out=G(5), in_=pb.ap[:, 0:24]), reads=[pb, gsm], writes=[gsm])
            for c in range(NCH):
                sl = slice(c * 6, (c + 1) * 6)
                P.op("dve", lambda e, sl=sl: e.tensor_tensor(out=G(6)[:, sl], in0=G(5)[:, sl], in1=mmst[l].ap, op=ALU.max),
                     reads=[gsm, mmst[l]], writes=[gsm])
                P.op("dve", lambda e, sl=sl: e.tensor_tensor(out=G(7)[:, sl], in0=mmst[l].ap, in1=G(6)[:, sl],
                                                             op=ALU.subtract), reads=[gsm, mmst[l]], writes=[gsm])
                P.op("dve", lambda e, sl=sl: e.tensor_tensor(out=mmst[l].ap, in0=G(6)[:, sl], in1=G(4)[:, sl],
                                                             op=ALU.subtract), reads=[gsm], writes=[mmst[l]])
            P.op("dve", lambda e: e.tensor_tensor(out=G(8), in0=G(2), in1=G(6), op=ALU.subtract), **RWg)
            P.op("act", lambda e: e.activation(out=G(8), in_=G(8), func=AF.Exp, bias=math.log(KSCALE), scale=1.0), **RWg)
            P.op("dve", lambda e: e.tensor_tensor(out=G(9), in0=G(3), in1=G(6), op=ALU.subtract), **RWg)
            P.op("act", lambda e: e.activation(out=G(9), in_=G(9), func=AF.Exp), **RWg)
            P.op("act", lambda e: e.activation(out=G(10), in_=G(7), func=AF.Exp), **RWg)
            for s in range(6):
                w = w_next(l, "tmm", s)
                isv = s < 3
                hh = (s % 3) * 2

                def evac(ps, cp, isv=isv, hh=hh):
                    for ci in range(2):
                        c = cp * 2 + ci
                        src = ps.ap[:, ci * 256:(ci + 1) * 256].rearrange("p (h v) -> p h v", v=128)
                        if isv:
                            P.op("dve", lambda e, c=c, src=src: e.tensor_copy(out=Vc[c].ap[:, hh:hh + 2, :], in_=src),
                                 reads=[ps], writes=[Vc[c]])
                        else:
                            P.op("act", lambda e, c=c, src=src: e.activation(out=Gc[c].ap[:, hh:hh + 2, :], in_=src,
                                                                             func=AF.Sigmoid),
                                 reads=[ps], writes=[Gc[c]])
                tm_strip(w, evac)
            ps_split(True)
            for c in range(NCH):
                for h in range(6):
                    k = h % 2
                    col = c * 6 + h
                    ps = ps_next()
                    P.op("pe", lambda e, h=h, ps=ps: e.matmul(ps.ap[:, 0:128], lhsT=mk[h].ap[:, cs(c)], rhs=mq[h].ap[:, cs(c)],
                                                            start=True, stop=True), reads=[mk[h], mq[h]], writes=[ps])
                    sp = spt[k]
                    P.op("dve", lambda e, ps=ps, sp=sp, col=col: e.scalar_tensor_tensor(
                        out=sp.ap, in0=ps.ap[:, 0:128], scalar=G(8)[:, col:col + 1], in1=tri_f.ap, op0=ALU.mult,
                        op1=ALU.mult), reads=[ps, gsm, tri_f], writes=[sp])
                    sb = stb[k]
                    P.op("pool", lambda e, h=h, sb=sb, col=col: e.tensor_scalar(
                        out=sb.ap[:, 0:129], in0=mstf[l][h].ap[:, 0:129], scalar1=G(10)[:, col:col + 1], scalar2=1.0,
                        op0=ALU.mult, op1=ALU.mult), reads=[mstf[l][h], gsm], writes=[sb])
                    po = ps_next(long=True)
                    P.op("pe", lambda e, h=h, po=po, sp=sp: e.matmul(po.ap[:, 0:128], lhsT=sp.ap, rhs=Vc[c].ap[:, h, :],
                                                                   start=True, stop=False), reads=[sp, Vc[c]], writes=[po])
                    P.op("pe", lambda e, h=h, po=po, sb=sb: e.matmul(po.ap[:, 0:128], lhsT=mq[h].ap[:, cs(c)],
                                                                   rhs=sb.ap[:, 0:128], start=False, stop=True),
                         reads=[mq[h], sb], writes=[po])
                    P.op("pe", lambda e, po=po, sp=sp: e.matmul(po.ap[:, 128:129], lhsT=sp.ap, rhs=ones_b.ap[:, 0:1],
                                                              start=True, stop=False), reads=[sp, ones_b], writes=[po])
                    P.op("pe", lambda e, h=h, po=po, sb=sb: e.matmul(po.ap[:, 128:129], lhsT=mq[h].ap[:, cs(c)],
                                                                   rhs=sb.ap[:, 128:129], start=False, stop=True),
                         reads=[mq[h], sb], writes=[po])
                    pk = ps_next()
                    pkv = pk.ap.bitcast(BF16)
                    P.op("pe", lambda e, h=h, pkv=pkv, pk=pk: e.transpose(pkv[:, 0:128], mk[h].ap[:, cs(c)], ident_b.ap),
                         reads=[mk[h], ident_b], writes=[pk])
                    ka = kd[k]
                    P.op("act", lambda e, pkv=pkv, pk=pk, ka=ka, col=col: e.activation(
                        out=ka.ap, in_=pkv[:, 0:128], func=AF.Identity, scale=G(8)[:, col:col + 1]),
                        reads=[pk, gsm], writes=[ka])
                    pv = ps_next()
                    P.op("pe", lambda e, h=h, pv=pv, ka=ka: e.matmul(pv.ap[:, 0:128], lhsT=ka.ap, rhs=Vc[c].ap[:, h, :],
                                                                   start=True, stop=True), reads=[ka, Vc[c]], writes=[pv])
                    P.op("pe", lambda e, pv=pv, ka=ka: e.matmul(pv.ap[:, 128:129], lhsT=ka.ap, rhs=ones_b.ap[:, 0:1],
                                                              start=True, stop=True), reads=[ka, ones_b], writes=[pv])
                    P.op("dve", lambda e, h=h, pv=pv, col=col: e.scalar_tensor_tensor(
                        out=mstf[l][h].ap[:, 0:129], in0=mstf[l][h].ap[:, 0:129], scalar=G(10)[:, col:col + 1],
                        in1=pv.ap[:, 0:129], op0=ALU.mult, op1=ALU.add), reads=[pv, mstf[l][h], gsm], writes=[mstf[l][h]])
                    dn = small2[(postn[0] + 1) % 2][3]
                    P.op("act", lambda e, po=po, col=col: e.activation(
                        out=dn.ap[:, 0:1], in_=po.ap[:, 128:129], func=AF.Abs), reads=[po], writes=[dn])
                    P.op("dve", lambda e, col=col: e.tensor_tensor(out=dn.ap[:, 0:1], in0=dn.ap[:, 0:1],
                                                                   in1=G(9)[:, col:col + 1], op=ALU.max),
                         reads=[dn, gsm], writes=[dn])
                    post(l, po, c, 6 + h, (Gc[c], Gc[c].ap[:, h, :]), den=dn)
            ps_split(False)

        DBG = getattr(cfg, "s5_dbg", 99)

        def s5(l, t):
            pstate["pool"] = [0, 1, 2, 3]
            yps = psb[4:8]
            tabb = [RA.view(i * 8192, [4, TT], F32) for i in range(2)]
            S_sets = [[RA.view(16384 + i * 2048, [TT], F32) for i in range(4)],
                      [RB.view(i * 2048, [TT], F32) for i in range(4)]]
            S = S_sets[0]
            rdts = [RA.view(40960, [TT], F32), RB.view(8192, [TT], F32)]
            srb = [RA.view(24576 + i * 1024, [TT], BF16) for i in range(2)]
            sib = [RA.view(26624 + i * 1024, [TT], BF16) for i in range(2)]
            suf = [RA.view(28672 + i * 2048, [TT], F32) for i in range(4)]
            sub = [RA.view(36864 + i * 1024, [TT], BF16) for i in range(4)]
            onesw = RA.view(43008, [TT], F32)
            s5sm = [RB.view(10240 + i * 256, [16], F32) for i in range(2)]
            if DBG > -2:
                P.op("pool", lambda e: e.memset(onesw.ap, 1.0), writes=[onesw])
            for s in range(2):
                w = w_next(l, "fms", s)
                if DBG <= -1:
                    continue
                for half in range(2):
                    ut = s * 2 + half
                    ps = ps_next()
                    fm_tile(w, half, ps)
                    P.op("act", lambda e, ps=ps, ut=ut: e.copy(out=suf[ut].ap, in_=ps.ap), reads=[ps], writes=[suf[ut]])
                    P.op("dve", lambda e, ps=ps, ut=ut: e.tensor_copy(out=sub[ut].ap, in_=ps.ap), reads=[ps], writes=[sub[ut]])

            def ld(tt_):
                tb = tabb[tt_ % 2]
                P.op("sp", lambda e: e.dma_start(out=tb.ap.rearrange("p a b -> p (a b)"), in_=tab_d[l, tt_]),
                     reads=[tabres[l][tt_]], writes=[tb], dma=True)
            if DBG > 0:
                ld(0)
            for tl in range(16):
                if DBG <= 0:
                    w_next(l, "s5w", tl)
                    continue
                if tl + 1 < 16:
                    ld(tl + 1)
                tb = tabb[tl % 2]
                S = S_sets[tl % 2]
                ut, j = tl // 4, tl % 4
                ww = w_next(l, "s5w", tl)
                wq = ww.ap[:, 0:512]
                pr, pi = ps_next(), ps_next()
                P.op("pe", lambda e: e.matmul(pr.ap, lhsT=wq[:, 0:128], rhs=sub[ut].ap,
                                              start=True, stop=True), reads=[ww, sub[ut]], writes=[pr])
                P.op("pe", lambda e: e.matmul(pi.ap, lhsT=wq[:, 128:256], rhs=sub[ut].ap,
                                              start=True, stop=True), reads=[ww, sub[ut]], writes=[pi])
                Mre, Mim, Cs, Sn = tb.ap[:, 0, :], tb.ap[:, 1, :], tb.ap[:, 2, :], tb.ap[:, 3, :]
                if DBG <= 1:
                    continue
                P.op("dve", lambda e: e.tensor_tensor(out=S[0].ap, in0=pr.ap, in1=Mre, op=ALU.mult), reads=[pr, tb], writes=[S[0]])
                P.op("dve", lambda e: e.tensor_tensor(out=S[1].ap, in0=pi.ap, in1=Mim, op=ALU.mult), reads=[pi, tb], writes=[S[1]])
                P.op("pool", lambda e: e.tensor_tensor(out=S[0].ap, in0=S[0].ap, in1=S[1].ap, op=ALU.subtract),
                     reads=[S[0], S[1]], writes=[S[0]])
                P.op("dve", lambda e: e.tensor_tensor(out=S[2].ap, in0=pi.ap, in1=Mre, op=ALU.mult), reads=[pi, tb], writes=[S[2]])
                P.op("dve", lambda e: e.tensor_tensor(out=S[3].ap, in0=pr.ap, in1=Mim, op=ALU.mult), reads=[pr, tb], writes=[S[3]])
                P.op("pool", lambda e: e.tensor_tensor(out=S[2].ap, in0=S[2].ap, in1=S[3].ap, op=ALU.add),
                     reads=[S[2], S[3]], writes=[S[2]])
                if DBG <= 2:
                    continue
                rdt = rdts[tl % 2]
                P.op("pool", lambda e: e.tensor_scalar(out=rdt.ap, in0=onesw.ap, scalar1=s5c.ap[:, l, 0, tl:tl + 1],
                                                       scalar2=1.0, op0=ALU.mult, op1=ALU.mult), reads=[onesw, s5c], writes=[rdt])
                rd = rdt.ap
                P.op("dve", lambda e: e.tensor_tensor_scan(out=S[1].ap, data0=rd, data1=S[0].ap,
                                                           initial=s5z.ap[:, l, 0, tl:tl + 1], op0=ALU.mult, op1=ALU.add),
                     reads=[S[0], rdt, s5z], writes=[S[1]])
                P.op("dve", lambda e: e.tensor_tensor_scan(out=S[3].ap, data0=rd, data1=S[2].ap,
                                                           initial=s5z.ap[:, l, 1, tl:tl + 1], op0=ALU.mult, op1=ALU.add),
                     reads=[S[2], rdt, s5z], writes=[S[3]])
                if DBG <= 3:
                    continue
                sm = s5sm[tl % 2]
                P.op("dve", lambda e: e.tensor_scalar(out=sm.ap[:, 0:1], in0=S[1].ap[:, TT - 1:TT],
                                                      scalar1=s5c.ap[:, l, 4, tl:tl + 1], scalar2=None, op0=ALU.mult),
                     reads=[S[1], s5c], writes=[sm])
                P.op("dve", lambda e: e.scalar_tensor_tensor(out=s5z.ap[:, l, 0, tl:tl + 1], in0=S[3].ap[:, TT - 1:TT],
                                                             scalar=s5c.ap[:, l, 6, tl:tl + 1], in1=sm.ap[:, 0:1],
                                                             op0=ALU.mult, op1=ALU.add),
                     reads=[S[3], s5c, sm], writes=[s5z])
                P.op("dve", lambda e: e.tensor_scalar(out=sm.ap[:, 1:2], in0=S[3].ap[:, TT - 1:TT],
                                                      scalar1=s5c.ap[:, l, 4, tl:tl + 1], scalar2=None, op0=ALU.mult),
                     reads=[S[3], s5c], writes=[sm])
                P.op("dve", lambda e: e.scalar_tensor_tensor(out=s5z.ap[:, l, 1, tl:tl + 1], in0=S[1].ap[:, TT - 1:TT],
                                                             scalar=s5c.ap[:, l, 5, tl:tl + 1], in1=sm.ap[:, 1:2],
                                                             op0=ALU.mult, op1=ALU.add),
                     reads=[S[1], s5c, sm], writes=[s5z])
                if DBG <= 4:
                    continue
                sr, si_ = srb[tl % 2], sib[tl % 2]
                P.op("pool", lambda e: e.tensor_tensor(out=S[0].ap, in0=S[1].ap, in1=Cs, op=ALU.mult), reads=[S[1], tb], writes=[S[0]])
                P.op("pool", lambda e: e.tensor_tensor(out=S[2].ap, in0=S[3].ap, in1=Sn, op=ALU.mult), reads=[S[3], tb], writes=[S[2]])
                P.op("dve", lambda e: e.tensor_tensor(out=sr.ap, in0=S[0].ap, in1=S[2].ap, op=ALU.subtract),
                     reads=[S[0], S[2]], writes=[sr])
                P.op("pool", lambda e: e.tensor_tensor(out=S[0].ap, in0=S[1].ap, in1=Sn, op=ALU.mult), reads=[S[1], tb], writes=[S[0]])
                P.op("pool", lambda e: e.tensor_tensor(out=S[2].ap, in0=S[3].ap, in1=Cs, op=ALU.mult), reads=[S[3], tb], writes=[S[2]])
                P.op("dve", lambda e: e.scalar_tensor_tensor(out=si_.ap, in0=S[0].ap, scalar=-1.0, in1=S[2].ap,
                                                             op0=ALU.mult, op1=ALU.subtract),
                     reads=[S[0], S[2]], writes=[si_])
                if DBG <= 5:
                    continue
                yv = yps[ut]
                P.op("pe", lambda e: e.matmul(yv.ap, lhsT=wq[:, 256:384], rhs=sr.ap, start=(j == 0), stop=False),
                     reads=[ww, sr], writes=[yv])
                P.op("pe", lambda e: e.matmul(yv.ap, lhsT=wq[:, 384:512], rhs=si_.ap, start=False, stop=(j == 3)),
                     reads=[ww, si_], writes=[yv])
            w = w_next(l, "glu", 0)
            if DBG <= 6:
                pstate["pool"] = list(range(8))
                for i in range(12, 16):
                    P.op("pool", lambda e, i=i: e.memset(mixT[i].ap, 0.0), writes=[mixT[i]])
                return
            wv = w.ap[:, 0:2048].rearrange("p (k c) -> p k c", c=512)
            S = S_sets[0]
            gf = [S[0], S[1], S[2], S[3]]
            gb = [srb[0], srb[1], sib[0], sib[1]]
            for ut in range(4):
                y = gf[ut]
                tmp = RA.view(ut % 2 * 2048, [TT], F32)
                P.op("dve", lambda e: e.scalar_tensor_tensor(out=y.ap, in0=suf[ut].ap, scalar=s5d.ap[:, l, ut:ut + 1],
                                                             in1=yps[ut].ap, op0=ALU.mult, op1=ALU.add),
                     reads=[suf[ut], s5d, yps[ut]], writes=[y])
                P.op("pool", lambda e: e.tensor_tensor(out=tmp.ap, in0=y.ap, in1=y.ap, op=ALU.mult), reads=[y], writes=[tmp])
                P.op("pool", lambda e: e.tensor_scalar(out=tmp.ap, in0=tmp.ap, scalar1=0.044715, scalar2=1.0, op0=ALU.mult,
                                                       op1=ALU.add), reads=[tmp], writes=[tmp])
                P.op("pool", lambda e: e.tensor_tensor(out=tmp.ap, in0=tmp.ap, in1=y.ap, op=ALU.mult), reads=[tmp, y], writes=[tmp])
                P.op("act", lambda e: e.activation(out=tmp.ap, in_=tmp.ap, func=AF.Sigmoid, scale=2.0 * math.sqrt(2.0 / math.pi)),
                     reads=[tmp], writes=[tmp])
                P.op("dve", lambda e: e.tensor_tensor(out=y.ap, in0=y.ap, in1=tmp.ap, op=ALU.mult), reads=[y, tmp], writes=[y])
                P.op("pool", lambda e: e.tensor_copy(out=gb[ut].ap, in_=y.ap), reads=[y], writes=[gb[ut]])
            pstate["pool"] = list(range(8))
            for mt in range(4):
                ps = ps_next()
                for kt in range(4):
                    P.op("pe", lambda e, kt=kt: e.matmul(ps.ap, lhsT=wv[:, kt, mt * 128:(mt + 1) * 128], rhs=gb[kt].ap,
                                                         start=(kt == 0), stop=(kt == 3)), reads=[w, gb[kt]], writes=[ps])
                tmp = RA.view(mt % 2 * 2048, [TT], F32)
                P.op("act", lambda e: e.activation(out=tmp.ap, in_=ps.ap, func=AF.Sigmoid, bias=glub.ap[:, l, mt:mt + 1],
                                                   scale=1.0), reads=[ps, glub], writes=[tmp])
                P.op("dve", lambda e: e.tensor_tensor(out=mixT[12 + mt].ap, in0=gf[mt].ap, in1=tmp.ap, op=ALU.mult),
                     reads=[gf[mt], tmp], writes=[mixT[12 + mt]])

        def mixer(l, t):
            rmsnorm_stats(sqb_m, rstd_m)
            apply_norm(2 * l, rstd_m)
            both = "ret" in cfg.parts and "ml" in cfg.parts
            if "ret" in cfg.parts:
                if both:
                    def inter(c):
                        for s in ((0, 1), (2, 3), (4, 5), ())[c]:
                            mlstm_fm(l, s)
                    retention(l, t, inter)
                else:
                    retention(l, t)
            else:
                for i in range(6):
                    P.op("pool", lambda e, i=i: e.memset(mixT[i].ap, 0.0), writes=[mixT[i]])
            if "ml" in cfg.parts:
                mlstm(l, t, fm_done=both)
            else:
                for i in range(6, 12):
                    P.op("pool", lambda e, i=i: e.memset(mixT[i].ap, 0.0), writes=[mixT[i]])
            if "s5" in cfg.parts and not getattr(cfg, "s5_skip_main", False):
                s5(l, t)
            else:
                for i in range(12, 16):
                    P.op("pool", lambda e, i=i: e.memset(mixT[i].ap, 0.0), writes=[mixT[i]])
            for s in range(8):
                w = w_next(l, "out", s)
                wv = w.ap.rearrange("p (k c) -> p k c", c=256)
                for half in range(2):
                    mt = s * 2 + half
                    ps = ps_next()
                    for kt in range(KT):
                        P.op("pe", lambda e, kt=kt: e.matmul(ps.ap, lhsT=wv[:, kt, half * 128:(half + 1) * 128],
                                                             rhs=mixT[kt].ap, start=(kt == 0), stop=(kt == KT - 1)),
                             reads=[w, mixT[kt]], writes=[ps])
                    P.op("dve", lambda e: e.tensor_tensor(out=xT[mt].ap, in0=ps.ap, in1=xT[mt].ap, op=ALU.add),
                         reads=[ps, xT[mt]], writes=[xT[mt]])

        sqb_m = [RB.view(4096, [TT], BF16), RB.view(5120, [TT], BF16)]
        rstd_m = RB.view(8192, [TT], F32)

        sqb = [RB.view(0, [TT], BF16), RB.view(1024, [TT], BF16)]
        rstd = RB.view(2048, [TT], F32)
        upsb = [[RB.view(4096 + (i * 2 + h) * 1032, [TT + 4], BF16) for h in range(2)] for i in range(2)]
        diag = [[[RB.view(8448 + ((i * 2 + h) * 3 + k) * 256, [128], BF16) for k in range(3)] for h in range(2)]
                for i in range(2)]
        sgb = [RB.view(11520 + i * 1024, [TT], BF16) for i in range(2)]
        xstage = [RB.view(14336 + i * 8192, [D], F32) for i in range(2)]

        xpref = set()

        def prefetch_x(t):
            if t >= cfg.ntile:
                return
            for c in range(2):
                r0 = t * TT + c * 128
                P.op("pool", lambda e, c=c, r0=r0: e.dma_start(out=xstage[c].ap, in_=x_d[r0:r0 + 128, :]),
                     writes=[xstage[c]], dma=True)
                xpref.add((t, c))

        def load_x(t):
            for c in range(NCH):
                stg = xstage[c % 2]
                r0 = t * TT + c * 128
                if (t, c) not in xpref:
                    P.op("pool", lambda e, stg=stg, r0=r0: e.dma_start(out=stg.ap, in_=x_d[r0:r0 + 128, :]),
                         writes=[stg], dma=True)
                for kt in range(KT):
                    ps = ps_next()
                    P.op("pe", lambda e, ps=ps, stg=stg, kt=kt: e.transpose(
                        ps.ap[:, 0:128], stg.ap[:, kt * 128:(kt + 1) * 128], ident_f.ap),
                        reads=[stg, ident_f], writes=[ps])
                    if kt % 2 == 0:
                        P.op("dve", lambda e, ps=ps, kt=kt, c=c: e.tensor_copy(
                            out=xT[kt].ap[:, cs(c)], in_=ps.ap[:, 0:128]), reads=[ps], writes=[xT[kt]])
                    else:
                        P.op("act", lambda e, ps=ps, kt=kt, c=c: e.copy(
                            out=xT[kt].ap[:, cs(c)], in_=ps.ap[:, 0:128]), reads=[ps], writes=[xT[kt]])

        def ffn(l, t):
            rmsnorm_stats(sqb, rstd)
            apply_norm(2 * l + 1, rstd)
            if l == cfg.depth - 1:
                prefetch_x(t + 1)
            def up_part(j):
                w = w_next(l, "up", j)
                wv = w.ap.rearrange("p (k c) -> p k c", c=256)
                ub = upsb[j % 2]
                dg = diag[j % 2]
                for h in range(2):
                    tile = j + h * NFF
                    ps = ps_next()
                    for kt in range(KT):
                        P.op("pe", lambda e, ps=ps, wv=wv, kt=kt, h=h: e.matmul(
                            ps.ap, lhsT=wv[:, kt, h * 128:(h + 1) * 128], rhs=hT[kt].ap,
                            start=(kt == 0), stop=(kt == KT - 1)), reads=[w, hT[kt]], writes=[ps])
                    u = ub[h]
                    P.op("pool", lambda e, u=u, tile=tile: e.tensor_copy(out=u.ap[:, 0:2], in_=fhist.ap[:, l, tile, :]),
                         reads=[fhist], writes=[u])
                    if h == 0:
                        P.op("dve", lambda e, u=u, ps=ps: e.tensor_copy(out=u.ap[:, 2:2 + TT], in_=ps.ap),
                             reads=[ps], writes=[u])
                    else:
                        P.op("act", lambda e, u=u, ps=ps: e.copy(out=u.ap[:, 2:2 + TT], in_=ps.ap),
                             reads=[ps], writes=[u])
                    P.op("pool", lambda e, u=u, tile=tile: e.tensor_copy(out=fhist.ap[:, l, tile, :],
                                                                       in_=u.ap[:, TT:TT + 2]),
                         reads=[u], writes=[fhist])
                    for k in range(3):
                        if (h * 3 + k) % 2 == 0:
                            P.op("dve", lambda e, d=dg[h][k], tile=tile, k=k: e.tensor_scalar(
                                out=d.ap, in0=ident_b.ap, scalar1=fcw.ap[:, l, tile, k:k + 1], scalar2=None,
                                op0=ALU.mult), reads=[ident_b, fcw], writes=[dg[h][k]])
                        else:
                            P.op("act", lambda e, d=dg[h][k], tile=tile, k=k: e.activation(
                                out=d.ap, in_=ident_b.ap, func=AF.Identity, scale=fcw.ap[:, l, tile, k:k + 1]),
                                reads=[ident_b, fcw], writes=[dg[h][k]])

            def conv_part(j):
                ub = upsb[j % 2]
                dg = diag[j % 2]
                pss = []
                for h in range(2):
                    u = ub[h]
                    ps2 = ps_next()
                    for k in range(3):
                        P.op("pe", lambda e, ps2=ps2, d=dg[h][k], u=u, k=k: e.matmul(
                            ps2.ap, lhsT=d.ap, rhs=u.ap[:, k:k + TT], start=(k == 0), stop=(k == 2)),
                            reads=[dg[h][k], u], writes=[ps2])
                    pss.append(ps2)
                sg = sgb[j % 2]
                P.op("act", lambda e, sg=sg, ps=pss[1], j=j: e.activation(
                    out=sg.ap, in_=ps.ap, func=AF.Silu, bias=fcb.ap[:, l, NFF + j:NFF + j + 1], scale=1.0),
                    reads=[pss[1], fcb], writes=[sg])
                P.op("dve", lambda e, sg=sg, ps=pss[0], j=j: e.scalar_tensor_tensor(
                    out=arena[j].ap, in0=ps.ap, scalar=fcb.ap[:, l, j:j + 1], in1=sg.ap,
                    op0=ALU.add, op1=ALU.mult), reads=[pss[0], fcb, sg], writes=[arena[j]])

            for j in range(NFF):
                up_part(j)
                if j >= 1:
                    conv_part(j - 1)
            conv_part(NFF - 1)
            for mt in range(KT):
                ps = ps_next()
                for kh in range(2):
                    w = w_next(l, "dn", mt * 2 + kh)
                    wv = w.ap[:, 0:2816].rearrange("p (k c) -> p k c", c=128)
                    for k in range(22):
                        kk = kh * 22 + k
                        P.op("pe", lambda e, ps=ps, wv=wv, k=k, kk=kk: e.matmul(
                            ps.ap, lhsT=wv[:, k, :], rhs=arena[kk].ap, start=(kk == 0), stop=(kk == NFF - 1)),
                            reads=[w, arena[kk]], writes=[ps])
                P.op("dve", lambda e, ps=ps, mt=mt: e.tensor_tensor(out=xT[mt].ap, in0=ps.ap, in1=xT[mt].ap,
                                                                   op=ALU.add),
                     reads=[ps, xT[mt]], writes=[xT[mt]])

        ostage = [RA.view(c * 8192, [D], F32) for c in range(NCH)]

        def final_out(t):
            rmsnorm_stats(sqb, rstd)
            ytile = [RB.view(4096 + i * 2048, [TT], F32) for i in range(2)]
            for kt in range(KT):
                y = ytile[kt % 2]
                P.op("dve", lambda e, y=y, kt=kt: e.scalar_tensor_tensor(
                    out=y.ap, in0=xT[kt].ap, scalar=nw.ap[:, 2 * cfg.depth, kt:kt + 1], in1=rstd.ap,
                    op0=ALU.mult, op1=ALU.mult), reads=[xT[kt], nw, rstd], writes=[y])
                for c in range(NCH):
                    ps = ps_next()
                    P.op("pe", lambda e, ps=ps, y=y, c=c: e.transpose(
                        ps.ap[:, 0:128], y.ap[:, cs(c)], ident_f.ap), reads=[y, ident_f], writes=[ps])
                    og = ostage[c]
                    if c % 2 == 0:
                        P.op("dve", lambda e, ps=ps, og=og, kt=kt: e.tensor_copy(
                            out=og.ap[:, kt * 128:(kt + 1) * 128], in_=ps.ap[:, 0:128]), reads=[ps], writes=[og])
                    else:
                        P.op("act", lambda e, ps=ps, og=og, kt=kt: e.copy(
                            out=og.ap[:, kt * 128:(kt + 1) * 128], in_=ps.ap[:, 0:128]), reads=[ps], writes=[og])
            for c in range(NCH):
                r0 = t * TT + c * 128
                P.op("pool", lambda e, og=ostage[c], r0=r0: e.dma_start(out=out_d[r0:r0 + 128, :], in_=og.ap),
                     reads=[ostage[c]], dma=True)

        for t in range(cfg.ntile):
            load_x(t)
            for l in range(cfg.depth):
                if cfg.mixer:
                    mixer(l, t)
                if cfg.ffn:
                    ffn(l, t)
            final_out(t)
        P.emit(nc, st)
    return nc


def prep_shared(inputs, cfg):
    m = {}
    f32 = np.float32
    fmr, fmm, fms, tm_ret, tm_ml, gates = win_plan()
    for l in range(cfg.depth):
        w_in = np.asarray(inputs["w_in"][l], f32)
        m[f"w_fmr_{l}"] = strips_from_cols(w_in, fmr, 256)
        m[f"w_fmm_{l}"] = strips_from_cols(w_in, fmm, 256)
        m[f"w_fms_{l}"] = strips_from_cols(w_in, fms, 256)
        m[f"w_tmr_{l}"] = strips_from_cols(w_in, tm_ret, 256)
        m[f"w_tmm_{l}"] = strips_from_cols(w_in, tm_ml, 256)
        m[f"w_gt_{l}"] = strips_from_cols(w_in, gates, 16)
        m[f"w_glu_{l}"] = strips_from_cols(np.asarray(inputs["s5_glu_w"][l], f32), list(range(512)), 512)
        m[f"w_out_{l}"] = strips_from_cols(np.asarray(inputs["w_out"][l], f32), list(range(D)), 256)
        upc = []
        for j in range(NFF):
            upc += list(range(j * 128, (j + 1) * 128)) + list(range(D_FF + j * 128, D_FF + (j + 1) * 128))
        m[f"w_up_{l}"] = strips_from_cols(np.asarray(inputs["ffn_w_up"][l], f32), upc, 256)
        wd = np.asarray(inputs["ffn_w_down"][l], f32)
        dn = np.zeros((32, 128, 2816), f32)
        for mt in range(16):
            for kh in range(2):
                blk = wd[kh * 2816:(kh + 1) * 2816, mt * 128:(mt + 1) * 128]
                dn[mt * 2 + kh] = blk.reshape(22, 128, 128).transpose(1, 0, 2).reshape(128, 2816)
        m[f"w_dn_{l}"] = dn
    nwl = []
    for l in range(cfg.depth):
        nwl += [per_part(inputs["norm1_w"][l], KT), per_part(inputs["norm2_w"][l], KT)]
    nwl.append(per_part(inputs["final_norm_w"], KT))
    m["nw"] = np.ascontiguousarray(np.stack(nwl, axis=1))
    m["fcw"] = np.ascontiguousarray(np.stack(
        [np.stack([per_part(inputs["ffn_conv_w"][l][k], 88) for k in range(3)], axis=-1) for l in range(cfg.depth)],
        axis=1))
    m["fcb"] = np.ascontiguousarray(np.stack([per_part(inputs["ffn_conv_b"][l], 88) for l in range(cfg.depth)], axis=1))
    m["ident"] = np.eye(128, dtype=f32)
    t = np.arange(cfg.seq, dtype=f32)
    inv = (np.float32(10000.0) ** (-np.arange(0, 64, 2, dtype=f32) / np.float32(64))).astype(f32)
    ang = (t[:, None] * inv[None, :]).astype(f32)
    cosv, sinv = np.cos(ang).astype(f32), np.sin(ang).astype(f32)
    rc = np.zeros((128, cfg.seq), f32)
    rs = np.zeros((128, cfg.seq), f32)
    for r in range(128):
        d = r % 64
        rc[r] = cosv[:, d % 32]
        rs[r] = -sinv[:, d % 32] if d < 32 else sinv[:, d % 32]
    m["ropec"], m["ropes"] = rc, rs
    lg = np.log1p(-np.exp2(-5.0 - np.arange(6, dtype=f32))).astype(f32)
    idx = np.arange(128, dtype=f32)
    rtab = np.zeros((128, 12, 128), f32)
    for h in range(6):
        diff = idx[None, :] - idx[:, None]
        rtab[:, h, :] = np.where(diff >= 0, np.exp(np.maximum(diff, 0.0) * lg[h]), 0.0).astype(f32)
    for i in range(3):
        for r in range(128):
            h = 2 * i + r // 64
            rtab[r, 6 + i, :] = np.exp((idx + 1.0) * lg[h]).astype(f32)
        for col in range(128):
            h = 2 * i + col // 64
            rtab[:, 9 + i, col] = np.exp((127.0 - idx) * lg[h]).astype(f32)
    m["rtab"] = rtab
    m["tri"] = np.triu(np.ones((128, 128), f32))
    m["mcw"] = np.ascontiguousarray(np.stack(
        [np.stack([per_part(inputs["mlstm_conv_w"][l][k], 12) for k in range(4)], axis=-1) for l in range(cfg.depth)],
        axis=1))
    m["mcb"] = np.ascontiguousarray(np.stack([per_part(inputs["mlstm_conv_b"][l], 12) for l in range(cfg.depth)], axis=1))
    m["gbias"] = np.ascontiguousarray(np.asarray(inputs["mlstm_gate_b"], f32)[:cfg.depth])
    m["gnw"] = np.ascontiguousarray(np.stack(
        [np.concatenate([per_part(inputs["ret_gn_w"][l], 6), per_part(inputs["mlstm_gn_w"][l], 6)], axis=1)
         for l in range(cfg.depth)], axis=1))
    s5a = np.zeros((128, cfg.depth, 3, 16), f32)
    for l in range(cfg.depth):
        s5a[:, l, 0] = per_part(np.asarray(inputs["s5_A_re"][l], f32).reshape(-1), 16)
        s5a[:, l, 1] = per_part(np.asarray(inputs["s5_A_im"][l], f32).reshape(-1), 16)
        s5a[:, l, 2] = per_part(np.repeat(np.asarray(inputs["s5_log_step"][l], f32), 64), 16)
        sw = np.zeros((16, 128, 4, 128), f32)
        for ri, (bn, cn) in enumerate((("s5_B_re", "s5_C_re"), ("s5_B_im", "s5_C_im"))):
            Bm = np.asarray(inputs[bn][l], f32)
            Cm = np.asarray(inputs[cn][l], f32)
            for g in range(32):
                gl8 = g % 8
                tl = g // 2
                sw[tl, gl8 * 16:(gl8 + 1) * 16, ri, (g % 2) * 64:(g % 2) * 64 + 64] = Bm[g].T
                sw[tl, (g % 2) * 64:(g % 2) * 64 + 64, 2 + ri, gl8 * 16:(gl8 + 1) * 16] = Cm[g].T
        m[f"w_s5w_{l}"] = np.ascontiguousarray(sw.reshape(16, 128, 512))
    m["s5a"] = s5a
    m["s5d"] = np.ascontiguousarray(np.stack([per_part(inputs["s5_D"][l], 4) for l in range(cfg.depth)], axis=1))
    m["glub"] = np.ascontiguousarray(np.stack([per_part(inputs["s5_glu_b"][l], 4) for l in range(cfg.depth)], axis=1))
    m["jidx"] = np.ascontiguousarray(np.broadcast_to(np.arange(516, dtype=f32)[None, :], (128, 516)))
    return m


_CACHE = {}


def run(inputs, cfg, ncores):
    key = (cfg.seq, cfg.depth, cfg.mixer, cfg.ffn, cfg.parts)
    if key not in _CACHE:
        _CACHE[key] = build(cfg)
    nc = _CACHE[key]
    shared = prep_shared(inputs, cfg)
    x = np.asarray(inputs["x"], np.float32)
    in_maps = []
    for c in range(ncores):
        mm = dict(shared)
        mm["x"] = np.ascontiguousarray(x[c, :cfg.seq])
        in_maps.append(mm)
    res = run_bass_kernel_spmd(nc, in_maps, core_ids=list(range(ncores)))
    return np.stack([res.results[c]["out"] for c in range(ncores)], axis=0)


def kernel(**inputs):
    cfg = Cfg()
    return run(inputs, cfg, 8).astype(np.float32)
```

```python
import math
from contextlib import ExitStack
import numpy as np
import concourse.bass as bass
import concourse.mybir as mybir
from concourse.bass_utils import run_bass_kernel_spmd

F32 = mybir.dt.float32
BF16 = mybir.dt.bfloat16
U8 = mybir.dt.uint8
I32 = mybir.dt.int32
AF = mybir.ActivationFunctionType
ALU = mybir.AluOpType
AX = mybir.AxisListType

D = 2048
KT = 16
SEQ = 4096
DEPTH = 2
TT = 512
NCH = 4
D_FF = 5632
NFF = 44
EPS = 1e-6
NQK = 384
D_IN = 5900


class Res:
    __slots__ = ("lw", "rd", "excl")

    def __init__(self, excl=False):
        self.lw = None
        self.rd = {}
        self.excl = excl


class Buf:
    def __init__(self, ap, cells):
        self.ap = ap
        self.cells = cells

    def __getitem__(self, idx):
        return self.ap[idx]


class Op:
    __slots__ = ("eng", "fn", "deps", "dma", "needed", "event")


COMPUTE = ("pe", "act", "dve", "pool")


class _Rec:
    def __getattr__(self, name):
        def f(*a, **k):
            self.call = (name, a, k)
            return self
        return f


class Prog:
    def __init__(self):
        self.ops = []

    def op(self, eng, fn, reads=(), writes=(), dma=False):
        i = len(self.ops)
        o = Op()
        rec = _Rec()
        fn(rec)
        o.eng, o.fn, o.dma, o.needed, o.event = eng, rec.call, dma, False, None
        deps = set()
        rcells = [c for b in reads for c in b.cells]
        wcells = [c for b in writes for c in b.cells]
        raw = set()
        key = ("dma", i) if dma else eng
        for c in rcells:
            if c.lw is not None:
                raw.add(c.lw)
            if c.excl:
                deps.update(v for kk, v in c.rd.items() if kk != key)
        for c in wcells:
            if c.lw is not None:
                deps.add(c.lw)
            deps.update(c.rd.values())
        deps |= raw
        deps.discard(i)
        fl = set()
        for d in deps:
            od = self.ops[d]
            if dma or od.dma or od.eng != eng:
                fl.add(d)
            elif eng != "pe" and d in raw:
                fl.add(d)
        o.deps = fl
        for d in fl:
            self.ops[d].needed = True
        for c in rcells:
            c.rd[key] = i
        for c in wcells:
            c.lw = i
            c.rd = {}
        self.ops.append(o)
        return i

    def emit(self, nc, stack, block_kwargs=None):
        ops = self.ops
        streams = {}
        for i, o in enumerate(ops):
            streams.setdefault(o.eng, []).append(i)
        ROT = 30000
        csem = {}
        ccnt = {e: 0 for e in streams}
        dsem = {}
        dcnt = {}
        NDS = {"sp": 12, "pool": 6, "act": 4}

        def getsem(name):
            return stack.enter_context(nc.semaphore(name))

        for e in streams:
            n_ev = sum(1 for i in streams[e] if ops[i].needed and not ops[i].dma)
            csem[e] = [getsem(f"c_{e}_{k}") for k in range(n_ev // ROT + 1)]
            if any(ops[i].dma for i in streams[e]):
                dsem[e] = [getsem(f"d_{e}_{k}") for k in range(NDS.get(e, 4))]
                dcnt[e] = [0] * len(dsem[e])
        rr = {e: 0 for e in streams}
        prevdma = {}
        for i, o in enumerate(ops):
            if o.dma:
                k = rr[o.eng] % len(dsem[o.eng])
                rr[o.eng] += 1
                dcnt[o.eng][k] += 16
                o.event = (dsem[o.eng][k], dcnt[o.eng][k], ("d", o.eng, k))
            elif o.needed:
                n = ccnt[o.eng]
                ccnt[o.eng] += 1
                o.event = (csem[o.eng][n // ROT], n % ROT + 1, ("c", o.eng, n // ROT))
        engmap = {"pe": "tensor", "act": "scalar", "dve": "vector", "pool": "gpsimd", "sp": "sync"}
        with nc.Block() as block:
            for ename, idxs in streams.items():
                def body(e, idxs=idxs):
                    known = {}
                    for i in idxs:
                        o = ops[i]
                        need = {}
                        for d in o.deps:
                            sem, val, key = ops[d].event
                            if known.get(key, 0) < val and need.get(key, (None, 0))[1] < val:
                                need[key] = (sem, val)
                        if o.dma:
                            sem, val, key = o.event
                            if val > 16 and known.get(key, 0) < val - 16 and need.get(key, (None, 0))[1] < val - 16:
                                need[key] = (sem, val - 16)
                        for key, (sem, val) in need.items():
                            e.wait_ge(sem, val)
                            known[key] = val
                        mname, ma, mk_ = o.fn
                        ins = getattr(e, mname)(*ma, **mk_)
                        if o.event is not None:
                            ins.then_inc(o.event[0], 16 if o.dma else 1)
                    if ename in dsem:
                        for k, s in enumerate(dsem[ename]):
                            if dcnt[ename][k] > 0 and known.get(("d", ename, k), 0) < dcnt[ename][k]:
                                e.wait_ge(s, dcnt[ename][k])
                getattr(block, engmap[ename])(body)


class Region:
    def __init__(self, nc, stack, name, nbytes, cell=512):
        self.t = stack.enter_context(nc.sbuf_tensor(name, [128, nbytes], U8))
        self.cell = cell
        self.n = nbytes
        self.cells = [Res() for _ in range((nbytes + cell - 1) // cell)]
        self.top = 0

    def view(self, off, shape, dtype):
        esz = {F32: 4, BF16: 2, I32: 4, U8: 1}[dtype]
        n = int(np.prod(shape)) * esz
        assert off % 4 == 0 and off + n <= self.n, (off, n, self.n)
        ap = self.t[:, off:off + n]
        if dtype != U8:
            ap = ap.bitcast(dtype)
        if len(shape) > 1:
            names = [f"d{i}" for i in range(len(shape))]
            kw = {names[i]: int(shape[i]) for i in range(1, len(shape))}
            ap = ap.rearrange(f"p ({' '.join(names)}) -> p {' '.join(names)}", **kw)
        return Buf(ap, self.cells[off // self.cell:(off + n - 1) // self.cell + 1])

    def alloc(self, shape, dtype):
        esz = {F32: 4, BF16: 2, I32: 4, U8: 1}[dtype]
        n = int(np.prod(shape)) * esz
        off = (self.top + self.cell - 1) // self.cell * self.cell
        self.top = off + n
        return self.view(off, shape, dtype)


def dram_buf(ap):
    return Buf(ap, [Res()])


def strips_from_cols(W, colidx, mc):
    K = W.shape[0]
    kt = K // 128
    colidx = np.asarray(colidx)
    ns = len(colidx) // mc
    Wp = np.concatenate([W, np.zeros((K, 1), W.dtype)], axis=1)
    G = Wp[:, colidx]
    G = G.reshape(kt, 128, ns, mc).transpose(2, 1, 0, 3)
    return np.ascontiguousarray(G.reshape(ns, 128, kt * mc))


def per_part(v, ntile):
    return np.ascontiguousarray(np.asarray(v, np.float32).reshape(ntile, 128).T)


def win_plan():
    rq0, rk0, rv0, rg0, mqk0, mv0, mo0, mg0, su0 = 0, 384, 768, 1536, 2304, 3840, 4608, 5376, 5388

    def plain(base, i):
        return list(range(base + i * 128, base + (i + 1) * 128))

    def swap(base, i):
        idx = []
        for h in (2 * i, 2 * i + 1):
            idx += list(range(base + h * 64 + 32, base + h * 64 + 64)) + list(range(base + h * 64, base + h * 64 + 32))
        return idx
    fmr = []
    for i in range(3):
        fmr += plain(rq0, i) + swap(rq0, i)
    for i in range(3):
        fmr += plain(rk0, i) + swap(rk0, i)
    fmm = list(range(mqk0, mqk0 + 1536))
    fms = list(range(su0, su0 + 512))
    tm_ret = list(range(rv0, rv0 + 768)) + list(range(rg0, rg0 + 768))
    tm_ml = list(range(mv0, mv0 + 768)) + list(range(mo0, mo0 + 768))
    gates = list(range(mg0, mg0 + 12)) + [-1] * 4
    return fmr, fmm, fms, tm_ret, tm_ml, gates


class Cfg:
    def __init__(self, seq=SEQ, depth=DEPTH, mixer=True, ffn=True, parts=("ret", "ml", "s5")):
        self.seq, self.depth, self.mixer, self.ffn, self.parts = seq, depth, mixer, ffn, tuple(parts)
        self.ntile = seq // TT


WKINDS = (("fmr", 6, 4096), ("tmr", 6, 4096), ("fmm", 6, 4096), ("gt", 1, 256), ("tmm", 6, 4096),
          ("fms", 2, 4096), ("s5w", 16, 512), ("glu", 1, 2048), ("out", 8, 4096), ("up", 44, 4096), ("dn", 32, 2816))
MIXK = ("fmr", "tmr", "fmm", "tmm", "gt", "fms", "s5w", "glu", "out")
KSCALE = 128.0 ** -0.5
TWO_PI = 2.0 * math.pi


def cs(c):
    return slice(c * 128, (c + 1) * 128)


def build(cfg):
    nc = bass.Bass("TRN2", target_bir_lowering=False)
    din = {}

    def inp(name, shape, dt=F32):
        din[name] = nc.dram_tensor(name, list(shape), dt, kind="ExternalInput").ap()
        return din[name]

    def kind_on(kind):
        if kind in ("up", "dn"):
            return cfg.ffn
        if not cfg.mixer:
            return False
        if kind in ("fmr", "tmr"):
            return "ret" in cfg.parts
        if kind in ("fmm", "tmm", "gt"):
            return "ml" in cfg.parts
        if kind in ("fms", "s5w", "glu"):
            return "s5" in cfg.parts and not getattr(cfg, "s5_skip_main", False)
        return True

    x_d = inp("x", [cfg.seq, D])
    out_d = nc.dram_tensor("out", [cfg.seq, D], F32, kind="ExternalOutput").ap()
    w32 = {}
    w16 = {}
    for l in range(cfg.depth):
        for kind, ns, ne in WKINDS:
            w32[l, kind] = inp(f"w_{kind}_{l}", [ns, 128, ne])
            w16[l, kind] = nc.dram_tensor(f"s_{kind}_{l}", [ns, 128, ne], BF16, kind="Internal").ap()
    nw_d = inp("nw", [128, 2 * cfg.depth + 1, KT])
    fcw_d = inp("fcw", [128, cfg.depth, 88, 3])
    fcb_d = inp("fcb", [128, cfg.depth, 88])
    ident_d = inp("ident", [128, 128])
    ropec_d = inp("ropec", [128, cfg.seq])
    ropes_d = inp("ropes", [128, cfg.seq])
    rtab_d = inp("rtab", [128, 12, 128])
    tri_d = inp("tri", [128, 128])
    mcw_d = inp("mcw", [128, cfg.depth, 12, 4])
    mcb_d = inp("mcb", [128, cfg.depth, 12])
    gbias_d = inp("gbias", [cfg.depth, 12])
    gnw_d = inp("gnw", [128, cfg.depth, 12])
    s5a_d = inp("s5a", [128, cfg.depth, 3, 16])
    s5d_d = inp("s5d", [128, cfg.depth, 4])
    glub_d = inp("glub", [128, cfg.depth, 4])
    jidx_d = inp("jidx", [128, 516])
    tab_d = nc.dram_tensor("s5tab", [cfg.depth, 16, 128, 2048], F32, kind="Internal").ap()
    GAM = [1.0 - 2.0 ** (-5.0 - h) for h in range(6)]
    GL = [float(np.exp(np.float32(128.0) * np.log1p(-np.exp2(np.float32(-5.0 - h))))) for h in range(6)]

    with ExitStack() as st0:
        P0 = Prog()
        R0 = Region(nc, st0, "r0", 4 * 16384 + 4 * 8192, cell=8192)
        sin = [R0.alloc([4096], F32) for _ in range(4)]
        sout = [R0.alloc([4096], BF16) for _ in range(4)]
        jobs = []
        for l in range(cfg.depth):
            for kind, ns, ne in WKINDS:
                if not kind_on(kind):
                    continue
                for s in range(ns):
                    jobs.append((w32[l, kind][s], w16[l, kind][s], ne))
        n = len(jobs)
        LOOK = 3

        def p0_load(i):
            src, dst, ne = jobs[i]
            a = sin[i % 4]
            P0.op("sp", lambda e: e.dma_start(out=a.ap[:, 0:ne], in_=src), writes=[a], dma=True)

        for i in range(min(LOOK, n)):
            p0_load(i)
        for i in range(n):
            src, dst, ne = jobs[i]
            a, b = sin[i % 4], sout[i % 4]
            if i % 2 == 0:
                P0.op("act", lambda e: e.copy(out=b.ap[:, 0:ne], in_=a.ap[:, 0:ne]), reads=[a], writes=[b])
            else:
                P0.op("dve", lambda e: e.tensor_copy(out=b.ap[:, 0:ne], in_=a.ap[:, 0:ne]), reads=[a], writes=[b])
            P0.op("pool", lambda e: e.dma_start(out=dst, in_=b.ap[:, 0:ne]), reads=[b], dma=True)
            if i + LOOK < n:
                p0_load(i + LOOK)
        if n:
            P0.emit(nc, st0)

    with ExitStack() as st:
        P = Prog()
        RP = Region(nc, st, "rp", 126 * 1024, cell=256)
        RA = Region(nc, st, "ra", 44 * 1024, cell=512)
        RB = Region(nc, st, "rb", 36 * 1024, cell=256)
        xT = [RP.alloc([TT], F32) for _ in range(KT)]
        hT = [RP.alloc([TT], BF16) for _ in range(KT)]
        NW = 3
        wb = [RP.alloc([4096], BF16) for _ in range(NW)]
        ident_f = RP.alloc([128], F32)
        ident_b = RP.alloc([128], BF16)
        ones_b = RP.alloc([128], BF16)
        ones_f = RP.alloc([128], F32)
        tri_f = RP.alloc([128], F32)
        nw = RP.alloc([2 * cfg.depth + 1, KT], F32)
        fcw = RP.alloc([cfg.depth, 88, 3], F32)
        fcb = RP.alloc([cfg.depth, 88], F32)
        fhist = RP.alloc([cfg.depth, 88, 2], BF16)
        rtab = RP.alloc([12, 128], F32)
        mcw = RP.alloc([cfg.depth, 12, 4], F32)
        mcb = RP.alloc([cfg.depth, 12], F32)
        mhist = RP.alloc([cfg.depth, 12, 4], BF16)
        gbias = RP.alloc([cfg.depth, 12], F32)
        gnw = RP.alloc([cfg.depth, 12], F32)
        nhalf = RP.alloc([1], F32)
        rstf = [[RP.alloc([128], F32) for h in range(6)] for l in range(cfg.depth)]
        rstb = [[RP.alloc([128], BF16) for h in range(6)] for l in range(cfg.depth)]
        mstf = [[RP.alloc([132], F32) for h in range(6)] for l in range(cfg.depth)]
        mmst = [RP.alloc([6], F32) for l in range(cfg.depth)]
        s5c = RP.alloc([cfg.depth, 8, 16], F32)
        s5z = RP.alloc([cfg.depth, 2, 16], F32)
        s5d = RP.alloc([cfg.depth, 4], F32)
        glub = RP.alloc([cfg.depth, 4], F32)
        arena = [RA.alloc([TT], BF16) for _ in range(44)]
        psb = [Buf(st.enter_context(nc.psum_tensor(f"ps{i}", [128, 512], F32))[:], [Res(excl=True)]) for i in range(8)]
        pstate = {"n": 0, "pool": list(range(8)), "ln": 0, "lpool": None}

        def ps_next(long=False):
            if long and pstate["lpool"]:
                lp = pstate["lpool"]
                b = psb[lp[pstate["ln"] % len(lp)]]
                pstate["ln"] += 1
                return b
            pool = pstate["pool"]
            b = psb[pool[pstate["n"] % len(pool)]]
            pstate["n"] += 1
            return b

        def ps_split(on):
            if on:
                pstate["pool"], pstate["lpool"] = [3, 4, 5, 6, 7], [0, 1, 2]
            else:
                pstate["pool"], pstate["lpool"] = list(range(8)), None

        def dma_in(dst, src, q="sp"):
            P.op(q, lambda e: e.dma_start(out=dst.ap, in_=src), writes=[dst], dma=True)

        dma_in(ident_f, ident_d)
        dma_in(nw, nw_d)
        dma_in(fcw, fcw_d)
        dma_in(fcb, fcb_d)
        dma_in(rtab, rtab_d)
        dma_in(tri_f, tri_d)
        dma_in(mcw, mcw_d)
        dma_in(mcb, mcb_d)
        dma_in(gnw, gnw_d)
        dma_in(s5d, s5d_d)
        dma_in(glub, glub_d)
        for l in range(cfg.depth):
            P.op("sp", lambda e, l=l: e.dma_start(out=gbias.ap[:, l, :], in_=gbias_d[l].partition_broadcast(128)),
                 writes=[gbias], dma=True)
        P.op("dve", lambda e: e.tensor_copy(out=ident_b.ap, in_=ident_f.ap), reads=[ident_f], writes=[ident_b])
        P.op("dve", lambda e: e.memset(ones_b.ap, 1.0), writes=[ones_b])
        P.op("dve", lambda e: e.memset(ones_f.ap, 1.0), writes=[ones_f])
        P.op("dve", lambda e: e.memset(nhalf.ap, -0.5), writes=[nhalf])
        P.op("dve", lambda e: e.memset(fhist.ap, 0.0), writes=[fhist])
        P.op("dve", lambda e: e.memset(mhist.ap, 0.0), writes=[mhist])
        P.op("dve", lambda e: e.memset(s5z.ap, 0.0), writes=[s5z])
        for l in range(cfg.depth):
            P.op("dve", lambda e, l=l: e.memset(mmst[l].ap, 0.0), writes=[mmst[l]])
            for h in range(6):
                P.op("pool", lambda e, b=rstf[l][h]: e.memset(b.ap, 0.0), writes=[rstf[l][h]])
                P.op("pool", lambda e, b=rstb[l][h]: e.memset(b.ap, 0.0), writes=[rstb[l][h]])
                P.op("pool", lambda e, b=mstf[l][h]: e.memset(b.ap, 0.0), writes=[mstf[l][h]])

        tabres = [[dram_buf(tab_d[l, t]) for t in range(16)] for l in range(cfg.depth)]

        def cossin(ang, width, ki, kf, sh, ch, co, si):
            P.op("dve", lambda e: e.tensor_scalar(out=ki, in0=ang, scalar1=1.0 / TWO_PI, scalar2=None, op0=ALU.mult),
                 reads=[SB], writes=[SB])
            P.op("dve", lambda e: e.tensor_copy(out=kf, in_=ki), reads=[SB], writes=[SB])
            P.op("dve", lambda e: e.scalar_tensor_tensor(out=kf, in0=kf, scalar=-TWO_PI, in1=ang, op0=ALU.mult,
                                                         op1=ALU.add), reads=[SB], writes=[SB])
            P.op("act", lambda e: e.activation(out=sh, in_=kf, func=AF.Sin, scale=0.5), reads=[SB], writes=[SB])
            P.op("act", lambda e: e.activation(out=kf, in_=kf, func=AF.Abs), reads=[SB], writes=[SB])
            P.op("act", lambda e: e.activation(out=ch, in_=kf, func=AF.Sin, scale=-0.5, bias=math.pi / 2),
                 reads=[SB], writes=[SB])
            P.op("dve", lambda e: e.tensor_tensor(out=co, in0=sh, in1=sh, op=ALU.mult), reads=[SB], writes=[SB])
            P.op("dve", lambda e: e.tensor_scalar(out=co, in0=co, scalar1=-2.0, scalar2=1.0, op0=ALU.mult, op1=ALU.add),
                 reads=[SB], writes=[SB])
            P.op("dve", lambda e: e.scalar_tensor_tensor(out=si, in0=sh, scalar=2.0, in1=ch, op0=ALU.mult,
                                                         op1=ALU.mult), reads=[SB], writes=[SB])

        if cfg.mixer and "s5" in cfg.parts:
            SB = RB.view(0, [7, 520], F32)
            SBi = RB.view(0, [7, 520], I32)
            s5a = RB.view(16384, [cfg.depth, 3, 16], F32)
            s5f = RB.view(17408, [2048], F32)
            jidx = RB.view(33792, [516], F32)
            tst = [RB.view(25600, [4, 512], F32)]
            dma_in(s5a, s5a_d)
            dma_in(jidx, jidx_d)
            for l in range(cfg.depth):
                ar, ai, ls = s5a.ap[:, l, 0, :], s5a.ap[:, l, 1, :], s5a.ap[:, l, 2, :]
                v = lambda i, w=16: SB.ap[:, i, 0:w]
                vi = lambda i, w=16: SBi.ap[:, i, 0:w]
                C = lambda i: s5c.ap[:, l, i, :]
                RW = dict(reads=[SB, s5a, s5c], writes=[SB, s5c])
                P.op("act", lambda e: e.activation(out=v(0), in_=ls, func=AF.Exp), **RW)
                P.op("dve", lambda e: e.tensor_tensor(out=v(1), in0=ar, in1=v(0), op=ALU.mult), **RW)
                P.op("act", lambda e: e.activation(out=C(0), in_=v(1), func=AF.Exp), **RW)
                P.op("dve", lambda e: e.tensor_tensor(out=v(1), in0=ai, in1=v(0), op=ALU.mult), **RW)
                cossin(v(1), 16, vi(2), v(2), v(3), v(4), v(5), v(6))
                P.op("dve", lambda e: e.tensor_scalar(out=vi(2), in0=v(1), scalar1=1.0 / TWO_PI, scalar2=None,
                                                      op0=ALU.mult), **RW)
                P.op("dve", lambda e: e.tensor_copy(out=v(3), in_=vi(2)), **RW)
                P.op("dve", lambda e: e.scalar_tensor_tensor(out=C(7), in0=v(3), scalar=-TWO_PI, in1=v(1),
                                                             op0=ALU.mult, op1=ALU.add), **RW)
                P.op("dve", lambda e: e.tensor_tensor(out=v(0), in0=C(0), in1=v(5), op=ALU.mult), **RW)
                P.op("dve", lambda e: e.tensor_scalar(out=v(0), in0=v(0), scalar1=-1.0, scalar2=None, op0=ALU.add), **RW)
                P.op("dve", lambda e: e.tensor_tensor(out=v(1), in0=C(0), in1=v(6), op=ALU.mult), **RW)
                P.op("dve", lambda e: e.tensor_tensor(out=v(2), in0=ar, in1=ar, op=ALU.mult), **RW)
                P.op("dve", lambda e: e.tensor_tensor(out=v(3), in0=ai, in1=ai, op=ALU.mult), **RW)
                P.op("dve", lambda e: e.tensor_tensor(out=v(2), in0=v(2), in1=v(3), op=ALU.add), **RW)
                P.op("dve", lambda e: e.reciprocal(out=v(2), in_=v(2)), **RW)
                P.op("dve", lambda e: e.tensor_tensor(out=v(3), in0=v(0), in1=ar, op=ALU.mult), **RW)
                P.op("dve", lambda e: e.tensor_tensor(out=v(4), in0=v(1), in1=ai, op=ALU.mult), **RW)
                P.op("dve", lambda e: e.tensor_tensor(out=v(3), in0=v(3), in1=v(4), op=ALU.add), **RW)
                P.op("dve", lambda e: e.tensor_tensor(out=C(1), in0=v(3), in1=v(2), op=ALU.mult), **RW)
                P.op("dve", lambda e: e.tensor_tensor(out=v(3), in0=v(1), in1=ar, op=ALU.mult), **RW)
                P.op("dve", lambda e: e.tensor_tensor(out=v(4), in0=v(0), in1=ai, op=ALU.mult), **RW)
                P.op("dve", lambda e: e.tensor_tensor(out=v(3), in0=v(3), in1=v(4), op=ALU.subtract), **RW)
                P.op("dve", lambda e: e.tensor_tensor(out=C(2), in0=v(3), in1=v(2), op=ALU.mult), **RW)
                P.op("dve", lambda e: e.tensor_scalar(out=C(3), in0=C(1), scalar1=-1.0, scalar2=None, op0=ALU.mult), **RW)
                for t in range(16):
                    W = 513
                    RW2 = dict(reads=[SB, s5c, jidx], writes=[SB])
                    P.op("dve", lambda e, t=t: e.tensor_scalar(out=v(0, W), in0=jidx.ap[:, 0:W],
                                                               scalar1=s5c.ap[:, l, 7, t:t + 1], scalar2=None,
                                                               op0=ALU.mult), **RW2)
                    cossin(v(0, W), W, vi(1, W), v(1, W), v(2, W), v(3, W), v(4, W), v(5, W))
                    tb = tst[0]
                    RW3 = dict(reads=[SB, s5c], writes=[SB, tb, s5c])
                    P.op("dve", lambda e, t=t: e.tensor_scalar(out=v(1, 512), in0=v(4, 512),
                                                               scalar1=s5c.ap[:, l, 1, t:t + 1], scalar2=None,
                                                               op0=ALU.mult), **RW3)
                    P.op("dve", lambda e, t=t, tb=tb: e.scalar_tensor_tensor(
                        out=tb.ap[:, 0, :], in0=v(5, 512), scalar=s5c.ap[:, l, 2, t:t + 1], in1=v(1, 512),
                        op0=ALU.mult, op1=ALU.add), **RW3)
                    P.op("dve", lambda e, t=t: e.tensor_scalar(out=v(1, 512), in0=v(4, 512),
                                                               scalar1=s5c.ap[:, l, 2, t:t + 1], scalar2=None,
                                                               op0=ALU.mult), **RW3)
                    P.op("dve", lambda e, t=t, tb=tb: e.scalar_tensor_tensor(
                        out=tb.ap[:, 1, :], in0=v(5, 512), scalar=s5c.ap[:, l, 3, t:t + 1], in1=v(1, 512),
                        op0=ALU.mult, op1=ALU.add), **RW3)
                    P.op("act", lambda e, tb=tb: e.copy(out=tb.ap[:, 2, :], in_=v(4, 512)), **RW3)
                    P.op("act", lambda e, tb=tb: e.copy(out=tb.ap[:, 3, :], in_=v(5, 512)), **RW3)
                    P.op("dve", lambda e, t=t: e.tensor_copy(out=s5c.ap[:, l, 4, t:t + 1], in_=v(4, W)[:, 512:513]), **RW3)
                    P.op("dve", lambda e, t=t: e.tensor_copy(out=s5c.ap[:, l, 5, t:t + 1], in_=v(5, W)[:, 512:513]), **RW3)
                    P.op("dve", lambda e, t=t: e.tensor_scalar(out=s5c.ap[:, l, 6, t:t + 1], in0=v(5, W)[:, 512:513],
                                                               scalar1=-1.0, scalar2=None, op0=ALU.mult), **RW3)
                    P.op("sp", lambda e, t=t, tb=tb: e.dma_start(
                        out=tab_d[l, t], in_=tb.ap.rearrange("p a b -> p (a b)")), reads=[tb], writes=[tabres[l][t]], dma=True)

        strip_seq = []
        for t in range(cfg.ntile):
            for l in range(cfg.depth):
                for kind, ns, ne in WKINDS:
                    if not kind_on(kind):
                        continue
                    for s in range(ns):
                        strip_seq.append((l, kind, s, ne))
        wstate = {"issued": 0, "used": 0}

        def w_issue():
            i = wstate["issued"]
            if i >= len(strip_seq):
                return
            l, kind, s, ne = strip_seq[i]
            slot = wb[i % NW]
            src = w16[l, kind][s]
            P.op("sp", lambda e, slot=slot, src=src, ne=ne: e.dma_start(out=slot.ap[:, 0:ne], in_=src),
                 writes=[slot], dma=True)
            wstate["issued"] += 1

        def w_next(l, kind, s):
            i = wstate["used"]
            assert strip_seq[i][:3] == (l, kind, s), (strip_seq[i], l, kind, s)
            while wstate["issued"] < min(i + NW, len(strip_seq)):
                w_issue()
            wstate["used"] += 1
            return wb[i % NW]

        def rmsnorm_stats(sqb, rstd):
            ps = ps_next()
            for kt in range(KT):
                sq = sqb[kt % 2]
                P.op("act", lambda e, sq=sq, kt=kt: e.activation(out=sq.ap, in_=xT[kt].ap, func=AF.Square),
                     reads=[xT[kt]], writes=[sq])
                P.op("pe", lambda e, sq=sq, kt=kt, ps=ps: e.matmul(ps.ap, lhsT=ones_b.ap, rhs=sq.ap,
                                                                 start=(kt == 0), stop=(kt == KT - 1)),
                     reads=[sq, ones_b], writes=[ps])
            P.op("act", lambda e, ps=ps: e.activation(out=rstd.ap, in_=ps.ap, func=AF.Sqrt, bias=EPS, scale=1.0 / D),
                 reads=[ps], writes=[rstd])
            P.op("dve", lambda e: e.reciprocal(out=rstd.ap, in_=rstd.ap), reads=[rstd], writes=[rstd])

        def apply_norm(nidx, rstd):
            for kt in range(KT):
                P.op("dve", lambda e, kt=kt: e.scalar_tensor_tensor(
                    out=hT[kt].ap, in0=xT[kt].ap, scalar=nw.ap[:, nidx, kt:kt + 1], in1=rstd.ap,
                    op0=ALU.mult, op1=ALU.mult), reads=[xT[kt], nw, rstd], writes=[hT[kt]])

        def fm_tile(w, half, ps):
            wv = w.ap.rearrange("p (k c) -> p k c", c=256)
            for kt in range(KT):
                P.op("pe", lambda e, kt=kt: e.matmul(ps.ap, lhsT=wv[:, kt, half * 128:(half + 1) * 128], rhs=hT[kt].ap,
                                                     start=(kt == 0), stop=(kt == KT - 1)),
                     reads=[w, hT[kt]], writes=[ps])

        def tm_strip(w, evac):
            wv = w.ap.rearrange("p (k c) -> p k c", c=256)
            for cp in range(2):
                ps = ps_next()
                for ci in range(2):
                    c = cp * 2 + ci
                    for kt in range(KT):
                        P.op("pe", lambda e, kt=kt, c=c, ci=ci, ps=ps: e.matmul(
                            ps.ap[:, ci * 256:(ci + 1) * 256], lhsT=hT[kt].ap[:, cs(c)], rhs=wv[:, kt, :],
                            start=(kt == 0), stop=(kt == KT - 1)), reads=[w, hT[kt]], writes=[ps])
                evac(ps, cp)

        ropec = RB.view(0, [TT], F32)
        ropes = RB.view(2048, [TT], F32)
        tA = [RB.view(4096 + i * 2048, [TT], F32) for i in range(2)]
        tB = [RB.view(8192 + i * 2048, [TT], F32) for i in range(2)]
        pcb = [RB.view(4096 + i * 1032, [TT + 4], BF16) for i in range(2)]
        mdiag = [[RB.view(8192 + (i * 4 + k) * 256, [128], BF16) for k in range(4)] for i in range(2)]
        spt = [RB.view(12288 + i * 256, [128], BF16) for i in range(2)]
        kd = [RB.view(12800 + i * 256, [128], BF16) for i in range(2)]
        t1 = [RB.view(13312 + i * 512, [128], F32) for i in range(2)]
        gG = [RB.view(14336 + i * 256, [128], BF16) for i in range(2)]
        stb = [RB.view(14848 + i * 264, [132], BF16) for i in range(2)]
        small2 = [[RB.view(2048 + (p_ * 4 + i) * 256, [16], F32) for i in range(4)] for p_ in range(2)]
        small = small2[0]
        gsm = RB.view(16384, [12, 24], F32)
        xg = RB.view(17664, [128], F32)
        mixT = [RB.view(20480 + i * 1024, [TT], BF16) for i in range(KT)]
        rq_rot, rq_dec, rk_rot, kpad = arena[0:3], arena[3:6], arena[6:9], arena[9:15]
        mq, mk = arena[15:21], arena[21:27]
        Vc = [None] * NCH
        for c in range(NCH):
            off = 27 * 1024 + c * 1536
            Vc[c] = RA.view(off, [6, 128], BF16)
        Gc = [RA.view(33 * 1024 + c * 1536, [6, 128], BF16) for c in range(NCH)]

        postn = [0]

        def post(l, ps, c, tile, gate_ap, den=None):
            postn[0] += 1
            k = postn[0] % 2
            sset = small2[postn[0] % 2]
            st6, mv, rs = sset[0], sset[1], sset[2]
            P.op("dve", lambda e: e.bn_stats(out=st6.ap[:, 0:6], in_=ps.ap[:, 0:128]), reads=[ps], writes=[st6])
            P.op("dve", lambda e: e.bn_aggr(out=mv.ap[:, 0:2], in_=st6.ap[:, 0:6]), reads=[st6], writes=[mv])
            if den is None:
                P.op("dve", lambda e: e.tensor_scalar(out=rs.ap[:, 0:1], in0=mv.ap[:, 1:2], scalar1=EPS, scalar2=None,
                                                      op0=ALU.add), reads=[mv], writes=[rs])
            else:
                P.op("dve", lambda e: e.tensor_tensor(out=rs.ap[:, 1:2], in0=den.ap[:, 0:1], in1=den.ap[:, 0:1],
                                                      op=ALU.mult), reads=[den], writes=[rs])
                P.op("dve", lambda e: e.scalar_tensor_tensor(out=rs.ap[:, 0:1], in0=rs.ap[:, 1:2], scalar=EPS,
                                                             in1=mv.ap[:, 1:2], op0=ALU.mult, op1=ALU.add),
                     reads=[rs, mv], writes=[rs])
            P.op("act", lambda e: e.activation(out=rs.ap[:, 3:4], in_=rs.ap[:, 0:1], func=AF.Sqrt), reads=[rs], writes=[rs])
            P.op("dve", lambda e: e.reciprocal(out=rs.ap[:, 2:3], in_=rs.ap[:, 3:4]), reads=[rs], writes=[rs])
            tt = t1[k]
            P.op("dve", lambda e: e.scalar_tensor_tensor(out=tt.ap, in0=ps.ap[:, 0:128], scalar=mv.ap[:, 0:1],
                                                         in1=gate_ap[1], op0=ALU.subtract, op1=ALU.mult),
                 reads=[ps, mv, gate_ap[0]], writes=[tt])
            g = gG[k]
            P.op("pool", lambda e: e.tensor_scalar(out=g.ap, in0=tt.ap, scalar1=rs.ap[:, 2:3], scalar2=1.0,
                                                   op0=ALU.mult, op1=ALU.mult), reads=[tt, rs], writes=[g])
            ps2 = ps_next()
            pv = ps2.ap.bitcast(BF16)
            P.op("pe", lambda e: e.transpose(pv[:, 0:128], g.ap, ident_b.ap), reads=[g, ident_b], writes=[ps2])
            P.op("act", lambda e: e.activation(out=mixT[tile].ap[:, cs(c)], in_=pv[:, 0:128], func=AF.Identity,
                                               scale=gnw.ap[:, l, tile:tile + 1]),
                 reads=[ps2, gnw], writes=[mixT[tile]])

        def retention(l, t, inter=None):
            P.op("pool", lambda e: e.dma_start(out=ropec.ap, in_=ropec_d[:, t * TT:(t + 1) * TT]), writes=[ropec], dma=True)
            P.op("pool", lambda e: e.dma_start(out=ropes.ap, in_=ropes_d[:, t * TT:(t + 1) * TT]), writes=[ropes], dma=True)
            for h in range(6):
                half = h % 2
                P.op("pool", lambda e, h=h, half=half: e.memset(kpad[h].ap[(1 - half) * 64:(2 - half) * 64, :], 0.0),
                     writes=[kpad[h]])
            for s in range(6):
                w = w_next(l, "fmr", s)
                psx, pss = ps_next(), ps_next()
                fm_tile(w, 0, psx)
                fm_tile(w, 1, pss)
                isq = s < 3
                i = s % 3
                a, b = tA[s % 2], tB[s % 2]
                sc = 0.125 if isq else 1.0
                P.op("dve", lambda e, a=a, psx=psx, sc=sc: e.scalar_tensor_tensor(
                    out=a.ap, in0=psx.ap, scalar=sc, in1=ropec.ap, op0=ALU.mult, op1=ALU.mult),
                    reads=[psx, ropec], writes=[a])
                P.op("dve", lambda e, b=b, pss=pss, sc=sc: e.scalar_tensor_tensor(
                    out=b.ap, in0=pss.ap, scalar=sc, in1=ropes.ap, op0=ALU.mult, op1=ALU.mult),
                    reads=[pss, ropes], writes=[b])
                dst = rq_rot[i] if isq else rk_rot[i]
                P.op("pool", lambda e, a=a, b=b, dst=dst: e.tensor_tensor(out=dst.ap, in0=a.ap, in1=b.ap, op=ALU.add),
                     reads=[a, b], writes=[dst])
                if isq:
                    for c in range(NCH):
                        P.op("pool", lambda e, c=c, i=i, dst=dst: e.tensor_tensor(
                            out=rq_dec[i].ap[:, cs(c)], in0=dst.ap[:, cs(c)], in1=rtab.ap[:, 6 + i, :], op=ALU.mult),
                            reads=[dst, rtab], writes=[rq_dec[i]])
                else:
                    for half in range(2):
                        P.op("pool", lambda e, i=i, half=half, dst=dst: e.tensor_copy(
                            out=kpad[2 * i + half].ap[half * 64:(half + 1) * 64, :],
                            in_=dst.ap[half * 64:(half + 1) * 64, :]), reads=[dst], writes=[kpad[2 * i + half]])
            for s in range(6):
                w = w_next(l, "tmr", s)
                isv = s < 3
                hh = (s % 3) * 2

                def evac(ps, cp, isv=isv, hh=hh):
                    for ci in range(2):
                        c = cp * 2 + ci
                        src = ps.ap[:, ci * 256:(ci + 1) * 256].rearrange("p (h v) -> p h v", v=128)
                        if isv:
                            P.op("dve", lambda e, c=c, src=src: e.tensor_copy(out=Vc[c].ap[:, hh:hh + 2, :], in_=src),
                                 reads=[ps], writes=[Vc[c]])
                        else:
                            P.op("act", lambda e, c=c, src=src: e.activation(out=Gc[c].ap[:, hh:hh + 2, :], in_=src,
                                                                             func=AF.Silu),
                                 reads=[ps], writes=[Gc[c]])
                tm_strip(w, evac)
            ps_split(True)
            for c in range(NCH):
                kds = []
                for i in range(3):
                    ps = ps_next()
                    pv = ps.ap.bitcast(BF16)
                    P.op("pe", lambda e, i=i, pv=pv: e.transpose(pv[:, 0:128], rk_rot[i].ap[:, cs(c)], ident_b.ap),
                         reads=[rk_rot[i], ident_b], writes=[ps])
                    kdi = RB.view(18432 + i * 256, [128], BF16)
                    P.op("dve", lambda e, i=i, pv=pv, kdi=kdi: e.tensor_tensor(
                        out=kdi.ap, in0=pv[:, 0:128], in1=rtab.ap[:, 9 + i, :], op=ALU.mult),
                        reads=[ps, rtab], writes=[kdi])
                    kds.append(kdi)
                for h in range(6):
                    i, half = h // 2, h % 2
                    k = h % 2
                    ps = ps_next()
                    P.op("pe", lambda e, h=h, i=i, ps=ps: e.matmul(ps.ap[:, 0:128], lhsT=kpad[h].ap[:, cs(c)],
                                                                 rhs=rq_rot[i].ap[:, cs(c)], start=True, stop=True),
                         reads=[kpad[h], rq_rot[i]], writes=[ps])
                    sp = spt[k]
                    P.op("dve", lambda e, h=h, ps=ps, sp=sp: e.tensor_tensor(out=sp.ap, in0=ps.ap[:, 0:128],
                                                                           in1=rtab.ap[:, h, :], op=ALU.mult),
                         reads=[ps, rtab], writes=[sp])
                    po = ps_next(long=True)
                    P.op("pe", lambda e, h=h, po=po, sp=sp: e.matmul(po.ap[:, 0:128], lhsT=sp.ap, rhs=Vc[c].ap[:, h, :],
                                                                   start=True, stop=False),
                         reads=[sp, Vc[c]], writes=[po])
                    P.op("pe", lambda e, h=h, i=i, po=po: e.matmul(po.ap[:, 0:128], lhsT=rq_dec[i].ap[:, cs(c)],
                                                                 rhs=rstb[l][h].ap, start=False, stop=True),
                         reads=[rq_dec[i], rstb[l][h]], writes=[po])
                    pk = ps_next()
                    P.op("pe", lambda e, h=h, i=i, pk=pk: e.matmul(pk.ap[:, 0:128], lhsT=kds[i].ap, rhs=Vc[c].ap[:, h, :],
                                                                 start=True, stop=True),
                         reads=[kds[i], Vc[c]], writes=[pk])
                    rows = slice(half * 64, (half + 1) * 64)
                    P.op("dve", lambda e, h=h, pk=pk, rows=rows: e.scalar_tensor_tensor(
                        out=rstf[l][h].ap[rows, :], in0=rstf[l][h].ap[rows, :], scalar=GL[h], in1=pk.ap[rows, 0:128],
                        op0=ALU.mult, op1=ALU.add), reads=[pk, rstf[l][h]], writes=[rstf[l][h]])
                    P.op("pool", lambda e, h=h, rows=rows: e.tensor_copy(out=rstb[l][h].ap[rows, :],
                                                                        in_=rstf[l][h].ap[rows, :]),
                         reads=[rstf[l][h]], writes=[rstb[l][h]])
                    post(l, po, c, h, (Gc[c], Gc[c].ap[:, h, :]))
                if inter is not None:
                    inter(c)
            ps_split(False)

        def mlstm_fm(l, s):
            if True:
                w = w_next(l, "fmm", s)
                for half in range(2):
                    tile = s * 2 + half
                    ps = ps_next()
                    fm_tile(w, half, ps)
                    pc = pcb[tile % 2]
                    dg = mdiag[tile % 2]
                    P.op("pool", lambda e, pc=pc, tile=tile: e.tensor_copy(out=pc.ap[:, 0:4], in_=mhist.ap[:, l, tile, :]),
                         reads=[mhist], writes=[pc])
                    P.op("act", lambda e, pc=pc, ps=ps: e.copy(out=pc.ap[:, 4:4 + TT], in_=ps.ap), reads=[ps], writes=[pc])
                    P.op("pool", lambda e, pc=pc, tile=tile: e.tensor_copy(out=mhist.ap[:, l, tile, :],
                                                                         in_=pc.ap[:, TT:TT + 4]),
                         reads=[pc], writes=[mhist])
                    for k in range(4):
                        if k % 2 == 0:
                            P.op("dve", lambda e, d=dg[k], tile=tile, k=k: e.tensor_scalar(
                                out=d.ap, in0=ident_b.ap, scalar1=mcw.ap[:, l, tile, k:k + 1], scalar2=None, op0=ALU.mult),
                                reads=[ident_b, mcw], writes=[dg[k]])
                        else:
                            P.op("act", lambda e, d=dg[k], tile=tile, k=k: e.activation(
                                out=d.ap, in_=ident_b.ap, func=AF.Identity, scale=mcw.ap[:, l, tile, k:k + 1]),
                                reads=[ident_b, mcw], writes=[dg[k]])
                    ps2 = ps_next()
                    for k in range(4):
                        P.op("pe", lambda e, ps2=ps2, d=dg[k], pc=pc, k=k: e.matmul(
                            ps2.ap, lhsT=d.ap, rhs=pc.ap[:, k + 1:k + 1 + TT], start=(k == 0), stop=(k == 3)),
                            reads=[dg[k], pc], writes=[ps2])
                    dst = mq[tile] if tile < 6 else mk[tile - 6]
                    P.op("act", lambda e, ps2=ps2, dst=dst, tile=tile: e.activation(
                        out=dst.ap, in_=ps2.ap, func=AF.Silu, bias=mcb.ap[:, l, tile:tile + 1], scale=1.0),
                        reads=[ps2, mcb], writes=[dst])

        def mlstm(l, t, fm_done=False):
            if not fm_done:
                for s in range(6):
                    mlstm_fm(l, s)
            w = w_next(l, "gt", 0)
            wv = w.ap[:, 0:256].rearrange("p (k c) -> p k c", c=16)
            psg = ps_next()
            for c in range(NCH):
                for kt in range(KT):
                    P.op("pe", lambda e, c=c, kt=kt: e.matmul(psg.ap[:, c * 16:(c + 1) * 16], lhsT=hT[kt].ap[:, cs(c)],
                                                              rhs=wv[:, kt, :], start=(kt == 0), stop=(kt == KT - 1)),
                         reads=[w, hT[kt]], writes=[psg])
            G = lambda i: gsm.ap[:, i, :]
            G3 = lambda i: gsm.ap[:, i, :].rearrange("p (c h) -> p c h", h=6)
            RWg = dict(reads=[gsm], writes=[gsm])
            pg3 = psg.ap[:, 0:64].rearrange("p (c g) -> p c g", g=16)
            for c in range(NCH):
                P.op("dve", lambda e, c=c: e.tensor_tensor(out=G3(0)[:, c, :], in0=pg3[:, c, 0:6], in1=gbias.ap[:, l, 0:6],
                                                           op=ALU.add), reads=[psg, gbias, gsm], writes=[gsm])
                P.op("dve", lambda e, c=c: e.tensor_tensor(out=G3(1)[:, c, :], in0=pg3[:, c, 6:12], in1=gbias.ap[:, l, 6:12],
                                                           op=ALU.add), reads=[psg, gbias, gsm], writes=[gsm])
            P.op("act", lambda e: e.activation(out=G(1), in_=G(1), func=AF.Exp, scale=-1.0), **RWg)
            P.op("act", lambda e: e.activation(out=G(1), in_=G(1), func=AF.Ln, bias=1.0, scale=1.0), **RWg)
            pp = ps_next()
            for c in range(NCH):
                P.op("pe", lambda e, c=c: e.matmul(pp.ap[:, c * 6:(c + 1) * 6], lhsT=tri_f.ap, rhs=G3(1)[:, c, :],
                                                   start=True, stop=True), reads=[tri_f, gsm], writes=[pp])
                P.op("pe", lambda e, c=c: e.matmul(pp.ap[:, 32 + c * 6:32 + (c + 1) * 6], lhsT=ones_f.ap, rhs=G3(1)[:, c, :],
                                                   start=True, stop=True), reads=[ones_f, gsm], writes=[pp])
            P.op("dve", lambda e: e.tensor_tensor(out=G(2), in0=pp.ap[:, 0:24], in1=G(0), op=ALU.add),
                 reads=[pp, gsm], writes=[gsm])
            P.op("dve", lambda e: e.tensor_copy(out=G(3), in_=pp.ap[:, 0:24]), reads=[pp, gsm], writes=[gsm])
            P.op("dve", lambda e: e.tensor_copy(out=G(4), in_=pp.ap[:, 32:56]), reads=[pp, gsm], writes=[gsm])
            pt = ps_next()
            P.op("pe", lambda e: e.transpose(pt.ap[0:24, 0:128], G(2), ident_f.ap), reads=[gsm, ident_f], writes=[pt])
            P.op("dve", lambda e: e.reduce_max(out=xg.ap[0:24, 0:1], in_=pt.ap[0:24, 0:128], axis=AX.X),
                 reads=[pt], writes=[xg])
            xg2 = RB.view(18176 + 1024, [128], F32)
            P.op("dve", lambda e: e.tensor_scalar(out=xg2.ap[0:24, :], in0=ones_f.ap[0:24, :], scalar1=xg.ap[0:24, 0:1],
                                                  scalar2=None, op0=ALU.mult), reads=[xg, ones_f], writes=[xg2])
            pb = ps_next()
            P.op("pe", lambda e: e.matmul(pb.ap[:, 0:24], lhsT=xg2.ap[0:24, :], rhs=ident_f.ap[0:24, 0:24],
                                          start=True, stop=True), reads=[xg2, ident_f], writes=[pb])
            P.op("dve", lambda e: e.tensor_copy(out=G(5), in_=pb.ap[:, 0:24]), reads=[pb, gsm], writes=[gsm])
            for c in range(NCH):
                sl = slice(c * 6, (c + 1) * 6)
                P.op("dve", lambda e, sl=sl: e.tensor_tensor(out=G(6)[:, sl], in0=G(5)[:, sl], in1=mmst[l].ap, op=ALU.max),
                     reads=[gsm, mmst[l]], writes=[gsm])
                P.op("dve", lambda e, sl=sl: e.tensor_tensor(out=G(7)[:, sl], in0=mmst[l].ap, in1=G(6)[:, sl],
                                                             op=ALU.subtract), reads=[gsm, mmst[l]], writes=[gsm])
                P.op("dve", lambda e, sl=sl: e.tensor_tensor(out=mmst[l].ap, in0=G(6)[:, sl], in1=G(4)[:, sl],
                                                             op=ALU.subtract), reads=[gsm], writes=[mmst[l]])
            P.op("dve", lambda e: e.tensor_tensor(out=G(8), in0=G(2), in1=G(6), op=ALU.subtract), **RWg)
            P.op("act", lambda e: e.activation(out=G(8), in_=G(8), func=AF.Exp, bias=math.log(KSCALE), scale=1.0), **RWg)
            P.op("dve", lambda e: e.tensor_tensor(out=G(9), in0=G(3), in1=G(6), op=ALU.subtract), **RWg)
            P.op("act", lambda e: e.activation(out=G(9), in_=G(9), func=AF.Exp), **RWg)
            P.op("act", lambda e: e.activation(out=G(10), in_=G(7), func=AF.Exp), **RWg)
            for s in range(6):
                w = w_next(l, "tmm", s)
                isv = s < 3
                hh = (s % 3) * 2

                def evac(ps, cp, isv=isv, hh=hh):
                    for ci in range(2):
                        c = cp * 2 + ci
                        src = ps.ap[:, ci * 256:(ci + 1) * 256].rearrange("p (h v) -> p h v", v=128)
                        if isv:
                            P.op("dve", lambda e, c=c, src=src: e.tensor_copy(out=Vc[c].ap[:, hh:hh + 2, :], in_=src),
                                 reads=[ps], writes=[Vc[c]])
                        else:
                            P.op("act", lambda e, c=c, src=src: e.activation(out=Gc[c].ap[:, hh:hh + 2, :], in_=src,
                                                                             func=AF.Sigmoid),
                                 reads=[ps], writes=[Gc[c]])
                tm_strip(w, evac)
            ps_split(True)
            for c in range(NCH):
                for h in range(6):
                    k = h % 2
                    col = c * 6 + h
                    ps = ps_next()
                    P.op("pe", lambda e, h=h, ps=ps: e.matmul(ps.ap[:, 0:128], lhsT=mk[h].ap[:, cs(c)], rhs=mq[h].ap[:, cs(c)],
                                                            start=True, stop=True), reads=[mk[h], mq[h]], writes=[ps])
                    sp = spt[k]
                    P.op("dve", lambda e, ps=ps, sp=sp, col=col: e.scalar_tensor_tensor(
                        out=sp.ap, in0=ps.ap[:, 0:128], scalar=G(8)[:, col:col + 1], in1=tri_f.ap, op0=ALU.mult,
                        op1=ALU.mult), reads=[ps, gsm, tri_f], writes=[sp])
                    sb = stb[k]
                    P.op("pool", lambda e, h=h, sb=sb, col=col: e.tensor_scalar(
                        out=sb.ap[:, 0:129], in0=mstf[l][h].ap[:, 0:129], scalar1=G(10)[:, col:col + 1], scalar2=1.0,
                        op0=ALU.mult, op1=ALU.mult), reads=[mstf[l][h], gsm], writes=[sb])
                    po = ps_next(long=True)
                    P.op("pe", lambda e, h=h, po=po, sp=sp: e.matmul(po.ap[:, 0:128], lhsT=sp.ap, rhs=Vc[c].ap[:, h, :],
                                                                   start=True, stop=False), reads=[sp, Vc[c]], writes=[po])
                    P.op("pe", lambda e, h=h, po=po, sb=sb: e.matmul(po.ap[:, 0:128], lhsT=mq[h].ap[:, cs(c)],
                                                                   rhs=sb.ap[:, 0:128], start=False, stop=True),
                         reads=[mq[h], sb], writes=[po])
                    P.op("pe", lambda e, po=po, sp=sp: e.matmul(po.ap[:, 128:129], lhsT=sp.ap, rhs=ones_b.ap[:, 0:1],
                                                              start=True, stop=False), reads=[sp, ones_b], writes=[po])
                    P.op("pe", lambda e, h=h, po=po, sb=sb: e.matmul(po.ap[:, 128:129], lhsT=mq[h].ap[:, cs(c)],
                                                                   rhs=sb.ap[:, 128:129], start=False, stop=True),
                         reads=[mq[h], sb], writes=[po])
                    pk = ps_next()
                    pkv = pk.ap.bitcast(BF16)
                    P.op("pe", lambda e, h=h, pkv=pkv, pk=pk: e.transpose(pkv[:, 0:128], mk[h].ap[:, cs(c)], ident_b.ap),
                         reads=[mk[h], ident_b], writes=[pk])
                    ka = kd[k]
                    P.op("act", lambda e, pkv=pkv, pk=pk, ka=ka, col=col: e.activation(
                        out=ka.ap, in_=pkv[:, 0:128], func=AF.Identity, scale=G(8)[:, col:col + 1]),
                        reads=[pk, gsm], writes=[ka])
                    pv = ps_next()
                    P.op("pe", lambda e, h=h, pv=pv, ka=ka: e.matmul(pv.ap[:, 0:128], lhsT=ka.ap, rhs=Vc[c].ap[:, h, :],
                                                                   start=True, stop=True), reads=[ka, Vc[c]], writes=[pv])
                    P.op("pe", lambda e, pv=pv, ka=ka: e.matmul(pv.ap[:, 128:129], lhsT=ka.ap, rhs=ones_b.ap[:, 0:1],
                                                              start=True, stop=True), reads=[ka, ones_b], writes=[pv])
                    P.op("dve", lambda e, h=h, pv=pv, col=col: e.scalar_tensor_tensor(
                        out=mstf[l][h].ap[:, 0:129], in0=mstf[l][h].ap[:, 0:129], scalar=G(10)[:, col:col + 1],
                        in1=pv.ap[:, 0:129], op0=ALU.mult, op1=ALU.add), reads=[pv, mstf[l][h], gsm], writes=[mstf[l][h]])
                    dn = small2[(postn[0] + 1) % 2][3]
                    P.op("act", lambda e, po=po, col=col: e.activation(
                        out=dn.ap[:, 0:1], in_=po.ap[:, 128:129], func=AF.Abs), reads=[po], writes=[dn])
                    P.op("dve", lambda e, col=col: e.tensor_tensor(out=dn.ap[:, 0:1], in0=dn.ap[:, 0:1],
                                                                   in1=G(9)[:, col:col + 1], op=ALU.max),
                         reads=[dn, gsm], writes=[dn])
                    post(l, po, c, 6 + h, (Gc[c], Gc[c].ap[:, h, :]), den=dn)
            ps_split(False)

        DBG = getattr(cfg, "s5_dbg", 99)

        def s5(l, t):
            pstate["pool"] = [0, 1, 2, 3]
            yps = psb[4:8]
            tabb = [RA.view(i * 8192, [4, TT], F32) for i in range(2)]
            S_sets = [[RA.view(16384 + i * 2048, [TT], F32) for i in range(4)],
                      [RB.view(i * 2048, [TT], F32) for i in range(4)]]
            S = S_sets[0]
            rdts = [RA.view(40960, [TT], F32), RB.view(8192, [TT], F32)]
            srb = [RA.view(24576 + i * 1024, [TT], BF16) for i in range(2)]
            sib = [RA.view(26624 + i * 1024, [TT], BF16) for i in range(2)]
            suf = [RA.view(28672 + i * 2048, [TT], F32) for i in range(4)]
            sub = [RA.view(36864 + i * 1024, [TT], BF16) for i in range(4)]
            onesw = RA.view(43008, [TT], F32)
            s5sm = [RB.view(10240 + i * 256, [16], F32) for i in range(2)]
            if DBG > -2:
                P.op("pool", lambda e: e.memset(onesw.ap, 1.0), writes=[onesw])
            for s in range(2):
                w = w_next(l, "fms", s)
                if DBG <= -1:
                    continue
                for half in range(2):
                    ut = s * 2 + half
                    ps = ps_next()
                    fm_tile(w, half, ps)
                    P.op("act", lambda e, ps=ps, ut=ut: e.copy(out=suf[ut].ap, in_=ps.ap), reads=[ps], writes=[suf[ut]])
                    P.op("dve", lambda e, ps=ps, ut=ut: e.tensor_copy(out=sub[ut].ap, in_=ps.ap), reads=[ps], writes=[sub[ut]])

            def ld(tt_):
                tb = tabb[tt_ % 2]
                P.op("sp", lambda e: e.dma_start(out=tb.ap.rearrange("p a b -> p (a b)"), in_=tab_d[l, tt_]),
                     reads=[tabres[l][tt_]], writes=[tb], dma=True)
            if DBG > 0:
                ld(0)
            for tl in range(16):
                if DBG <= 0:
                    w_next(l, "s5w", tl)
                    continue
                if tl + 1 < 16:
                    ld(tl + 1)
                tb = tabb[tl % 2]
                S = S_sets[tl % 2]
                ut, j = tl // 4, tl % 4
                ww = w_next(l, "s5w", tl)
                wq = ww.ap[:, 0:512]
                pr, pi = ps_next(), ps_next()
                P.op("pe", lambda e: e.matmul(pr.ap, lhsT=wq[:, 0:128], rhs=sub[ut].ap,
                                              start=True, stop=True), reads=[ww, sub[ut]], writes=[pr])
                P.op("pe", lambda e: e.matmul(pi.ap, lhsT=wq[:, 128:256], rhs=sub[ut].ap,
                                              start=True, stop=True), reads=[ww, sub[ut]], writes=[pi])
                Mre, Mim, Cs, Sn = tb.ap[:, 0, :], tb.ap[:, 1, :], tb.ap[:, 2, :], tb.ap[:, 3, :]
                if DBG <= 1:
                    continue
                P.op("dve", lambda e: e.tensor_tensor(out=S[0].ap, in0=pr.ap, in1=Mre, op=ALU.mult), reads=[pr, tb], writes=[S[0]])
                P.op("dve", lambda e: e.tensor_tensor(out=S[1].ap, in0=pi.ap, in1=Mim, op=ALU.mult), reads=[pi, tb], writes=[S[1]])
                P.op("pool", lambda e: e.tensor_tensor(out=S[0].ap, in0=S[0].ap, in1=S[1].ap, op=ALU.subtract),
                     reads=[S[0], S[1]], writes=[S[0]])
                P.op("dve", lambda e: e.tensor_tensor(out=S[2].ap, in0=pi.ap, in1=Mre, op=ALU.mult), reads=[pi, tb], writes=[S[2]])
                P.op("dve", lambda e: e.tensor_tensor(out=S[3].ap, in0=pr.ap, in1=Mim, op=ALU.mult), reads=[pr, tb], writes=[S[3]])
                P.op("pool", lambda e: e.tensor_tensor(out=S[2].ap, in0=S[2].ap, in1=S[3].ap, op=ALU.add),
                     reads=[S[2], S[3]], writes=[S[2]])
                if DBG <= 2:
                    continue
                rdt = rdts[tl % 2]
                P.op("pool", lambda e: e.tensor_scalar(out=rdt.ap, in0=onesw.ap, scalar1=s5c.ap[:, l, 0, tl:tl + 1],
                                                       scalar2=1.0, op0=ALU.mult, op1=ALU.mult), reads=[onesw, s5c], writes=[rdt])
                rd = rdt.ap
                P.op("dve", lambda e: e.tensor_tensor_scan(out=S[1].ap, data0=rd, data1=S[0].ap,
                                                           initial=s5z.ap[:, l, 0, tl:tl + 1], op0=ALU.mult, op1=ALU.add),
                     reads=[S[0], rdt, s5z], writes=[S[1]])
                P.op("dve", lambda e: e.tensor_tensor_scan(out=S[3].ap, data0=rd, data1=S[2].ap,
                                                           initial=s5z.ap[:, l, 1, tl:tl + 1], op0=ALU.mult, op1=ALU.add),
                     reads=[S[2], rdt, s5z], writes=[S[3]])
                if DBG <= 3:
                    continue
                sm = s5sm[tl % 2]
                P.op("dve", lambda e: e.tensor_scalar(out=sm.ap[:, 0:1], in0=S[1].ap[:, TT - 1:TT],
                                                      scalar1=s5c.ap[:, l, 4, tl:tl + 1], scalar2=None, op0=ALU.mult),
                     reads=[S[1], s5c], writes=[sm])
                P.op("dve", lambda e: e.scalar_tensor_tensor(out=s5z.ap[:, l, 0, tl:tl + 1], in0=S[3].ap[:, TT - 1:TT],
                                                             scalar=s5c.ap[:, l, 6, tl:tl + 1], in1=sm.ap[:, 0:1],
                                                             op0=ALU.mult, op1=ALU.add),
                     reads=[S[3], s5c, sm], writes=[s5z])
                P.op("dve", lambda e: e.tensor_scalar(out=sm.ap[:, 1:2], in0=S[3].ap[:, TT - 1:TT],
                                                      scalar1=s5c.ap[:, l, 4, tl:tl + 1], scalar2=None, op0=ALU.mult),
                     reads=[S[3], s5c], writes=[sm])
                P.op("dve", lambda e: e.scalar_tensor_tensor(out=s5z.ap[:, l, 1, tl:tl + 1], in0=S[1].ap[:, TT - 1:TT],
                                                             scalar=s5c.ap[:, l, 5, tl:tl + 1], in1=sm.ap[:, 1:2],
                                                             op0=ALU.mult, op1=ALU.add),
                     reads=[S[1], s5c, sm], writes=[s5z])
                if DBG <= 4:
                    continue
                sr, si_ = srb[tl % 2], sib[tl % 2]
                P.op("pool", lambda e: e.tensor_tensor(out=S[0].ap, in0=S[1].ap, in1=Cs, op=ALU.mult), reads=[S[1], tb], writes=[S[0]])
                P.op("pool", lambda e: e.tensor_tensor(out=S[2].ap, in0=S[3].ap, in1=Sn, op=ALU.mult), reads=[S[3], tb], writes=[S[2]])
                P.op("dve", lambda e: e.tensor_tensor(out=sr.ap, in0=S[0].ap, in1=S[2].ap, op=ALU.subtract),
                     reads=[S[0], S[2]], writes=[sr])
                P.op("pool", lambda e: e.tensor_tensor(out=S[0].ap, in0=S[1].ap, in1=Sn, op=ALU.mult), reads=[S[1], tb], writes=[S[0]])
                P.op("pool", lambda e: e.tensor_tensor(out=S[2].ap, in0=S[3].ap, in1=Cs, op=ALU.mult), reads=[S[3], tb], writes=[S[2]])
                P.op("dve", lambda e: e.scalar_tensor_tensor(out=si_.ap, in0=S[0].ap, scalar=-1.0, in1=S[2].ap,
                                                             op0=ALU.mult, op1=ALU.subtract),
                     reads=[S[0], S[2]], writes=[si_])
                if DBG <= 5:
                    continue
                yv = yps[ut]
                P.op("pe", lambda e: e.matmul(yv.ap, lhsT=wq[:, 256:384], rhs=sr.ap, start=(j == 0), stop=False),
                     reads=[ww, sr], writes=[yv])
                P.op("pe", lambda e: e.matmul(yv.ap, lhsT=wq[:, 384:512], rhs=si_.ap, start=False, stop=(j == 3)),
                     reads=[ww, si_], writes=[yv])
            w = w_next(l, "glu", 0)
            if DBG <= 6:
                pstate["pool"] = list(range(8))
                for i in range(12, 16):
                    P.op("pool", lambda e, i=i: e.memset(mixT[i].ap, 0.0), writes=[mixT[i]])
                return
            wv = w.ap[:, 0:2048].rearrange("p (k c) -> p k c", c=512)
            S = S_sets[0]
            gf = [S[0], S[1], S[2], S[3]]
            gb = [srb[0], srb[1], sib[0], sib[1]]
            for ut in range(4):
                y = gf[ut]
                tmp = RA.view(ut % 2 * 2048, [TT], F32)
                P.op("dve", lambda e: e.scalar_tensor_tensor(out=y.ap, in0=suf[ut].ap, scalar=s5d.ap[:, l, ut:ut + 1],
                                                             in1=yps[ut].ap, op0=ALU.mult, op1=ALU.add),
                     reads=[suf[ut], s5d, yps[ut]], writes=[y])
                P.op("pool", lambda e: e.tensor_tensor(out=tmp.ap, in0=y.ap, in1=y.ap, op=ALU.mult), reads=[y], writes=[tmp])
                P.op("pool", lambda e: e.tensor_scalar(out=tmp.ap, in0=tmp.ap, scalar1=0.044715, scalar2=1.0, op0=ALU.mult,
                                                       op1=ALU.add), reads=[tmp], writes=[tmp])
                P.op("pool", lambda e: e.tensor_tensor(out=tmp.ap, in0=tmp.ap, in1=y.ap, op=ALU.mult), reads=[tmp, y], writes=[tmp])
                P.op("act", lambda e: e.activation(out=tmp.ap, in_=tmp.ap, func=AF.Sigmoid, scale=2.0 * math.sqrt(2.0 / math.pi)),
                     reads=[tmp], writes=[tmp])
                P.op("dve", lambda e: e.tensor_tensor(out=y.ap, in0=y.ap, in1=tmp.ap, op=ALU.mult), reads=[y, tmp], writes=[y])
                P.op("pool", lambda e: e.tensor_copy(out=gb[ut].ap, in_=y.ap), reads=[y], writes=[gb[ut]])
            pstate["pool"] = list(range(8))
            for mt in range(4):
                ps = ps_next()
                for kt in range(4):
                    P.op("pe", lambda e, kt=kt: e.matmul(ps.ap, lhsT=wv[:, kt, mt * 128:(mt + 1) * 128], rhs=gb[kt].ap,
                                                         start=(kt == 0), stop=(kt == 3)), reads=[w, gb[kt]], writes=[ps])
                tmp = RA.view(mt % 2 * 2048, [TT], F32)
                P.op("act", lambda e: e.activation(out=tmp.ap, in_=ps.ap, func=AF.Sigmoid, bias=glub.ap[:, l, mt:mt + 1],
                                                   scale=1.0), reads=[ps, glub], writes=[tmp])
                P.op("dve", lambda e: e.tensor_tensor(out=mixT[12 + mt].ap, in0=gf[mt].ap, in1=tmp.ap, op=ALU.mult),
                     reads=[gf[mt], tmp], writes=[mixT[12 + mt]])

        def mixer(l, t):
            rmsnorm_stats(sqb_m, rstd_m)
            apply_norm(2 * l, rstd_m)
            both = "ret" in cfg.parts and "ml" in cfg.parts
            if "ret" in cfg.parts:
                if both:
                    def inter(c):
                        for s in ((0, 1), (2, 3), (4, 5), ())[c]:
                            mlstm_fm(l, s)
                    retention(l, t, inter)
                else:
                    retention(l, t)
            else:
                for i in range(6):
                    P.op("pool", lambda e, i=i: e.memset(mixT[i].ap, 0.0), writes=[mixT[i]])
            if "ml" in cfg.parts:
                mlstm(l, t, fm_done=both)
            else:
                for i in range(6, 12):
                    P.op("pool", lambda e, i=i: e.memset(mixT[i].ap, 0.0), writes=[mixT[i]])
            if "s5" in cfg.parts and not getattr(cfg, "s5_skip_main", False):
                s5(l, t)
            else:
                for i in range(12, 16):
                    P.op("pool", lambda e, i=i: e.memset(mixT[i].ap, 0.0), writes=[mixT[i]])
            for s in range(8):
                w = w_next(l, "out", s)
                wv = w.ap.rearrange("p (k c) -> p k c", c=256)
                for half in range(2):
                    mt = s * 2 + half
                    ps = ps_next()
                    for kt in range(KT):
                        P.op("pe", lambda e, kt=kt: e.matmul(ps.ap, lhsT=wv[:, kt, half * 128:(half + 1) * 128],
                                                             rhs=mixT[kt].ap, start=(kt == 0), stop=(kt == KT - 1)),
                             reads=[w, mixT[kt]], writes=[ps])
                    P.op("dve", lambda e: e.tensor_tensor(out=xT[mt].ap, in0=ps.ap, in1=xT[mt].ap, op=ALU.add),
                         reads=[ps, xT[mt]], writes=[xT[mt]])

        sqb_m = [RB.view(4096, [TT], BF16), RB.view(5120, [TT], BF16)]
        rstd_m = RB.view(8192, [TT], F32)

        sqb = [RB.view(0, [TT], BF16), RB.view(1024, [TT], BF16)]
        rstd = RB.view(2048, [TT], F32)
        upsb = [[RB.view(4096 + (i * 2 + h) * 1032, [TT + 4], BF16) for h in range(2)] for i in range(2)]
        diag = [[[RB.view(8448 + ((i * 2 + h) * 3 + k) * 256, [128], BF16) for k in range(3)] for h in range(2)]
                for i in range(2)]
        sgb = [RB.view(11520 + i * 1024, [TT], BF16) for i in range(2)]
        xstage = [RB.view(14336 + i * 8192, [D], F32) for i in range(2)]

        xpref = set()

        def prefetch_x(t):
            if t >= cfg.ntile:
                return
            for c in range(2):
                r0 = t * TT + c * 128
                P.op("pool", lambda e, c=c, r0=r0: e.dma_start(out=xstage[c].ap, in_=x_d[r0:r0 + 128, :]),
                     writes=[xstage[c]], dma=True)
                xpref.add((t, c))

        def load_x(t):
            for c in range(NCH):
                stg = xstage[c % 2]
                r0 = t * TT + c * 128
                if (t, c) not in xpref:
                    P.op("pool", lambda e, stg=stg, r0=r0: e.dma_start(out=stg.ap, in_=x_d[r0:r0 + 128, :]),
                         writes=[stg], dma=True)
                for kt in range(KT):
                    ps = ps_next()
                    P.op("pe", lambda e, ps=ps, stg=stg, kt=kt: e.transpose(
                        ps.ap[:, 0:128], stg.ap[:, kt * 128:(kt + 1) * 128], ident_f.ap),
                        reads=[stg, ident_f], writes=[ps])
                    if kt % 2 == 0:
                        P.op("dve", lambda e, ps=ps, kt=kt, c=c: e.tensor_copy(
                            out=xT[kt].ap[:, cs(c)], in_=ps.ap[:, 0:128]), reads=[ps], writes=[xT[kt]])
                    else:
                        P.op("act", lambda e, ps=ps, kt=kt, c=c: e.copy(
                            out=xT[kt].ap[:, cs(c)], in_=ps.ap[:, 0:128]), reads=[ps], writes=[xT[kt]])

        def ffn(l, t):
            rmsnorm_stats(sqb, rstd)
            apply_norm(2 * l + 1, rstd)
            def up_part(j):
                w = w_next(l, "up", j)
                wv = w.ap.rearrange("p (k c) -> p k c", c=256)
                ub = upsb[j % 2]
                dg = diag[j % 2]
                for h in range(2):
                    tile = j + h * NFF
                    ps = ps_next()
                    for kt in range(KT):
                        P.op("pe", lambda e, ps=ps, wv=wv, kt=kt, h=h: e.matmul(
                            ps.ap, lhsT=wv[:, kt, h * 128:(h + 1) * 128], rhs=hT[kt].ap,
                            start=(kt == 0), stop=(kt == KT - 1)), reads=[w, hT[kt]], writes=[ps])
                    u = ub[h]
                    P.op("pool", lambda e, u=u, tile=tile: e.tensor_copy(out=u.ap[:, 0:2], in_=fhist.ap[:, l, tile, :]),
                         reads=[fhist], writes=[u])
                    if h == 0:
                        P.op("dve", lambda e, u=u, ps=ps: e.tensor_copy(out=u.ap[:, 2:2 + TT], in_=ps.ap),
                             reads=[ps], writes=[u])
                    else:
                        P.op("act", lambda e, u=u, ps=ps: e.copy(out=u.ap[:, 2:2 + TT], in_=ps.ap),
                             reads=[ps], writes=[u])
                    P.op("pool", lambda e, u=u, tile=tile: e.tensor_copy(out=fhist.ap[:, l, tile, :],
                                                                       in_=u.ap[:, TT:TT + 2]),
                         reads=[u], writes=[fhist])
                    for k in range(3):
                        if (h * 3 + k) % 2 == 0:
                            P.op("dve", lambda e, d=dg[h][k], tile=tile, k=k: e.tensor_scalar(
                                out=d.ap, in0=ident_b.ap, scalar1=fcw.ap[:, l, tile, k:k + 1], scalar2=None,
                                op0=ALU.mult), reads=[ident_b, fcw], writes=[dg[h][k]])
                        else:
                            P.op("act", lambda e, d=dg[h][k], tile=tile, k=k: e.activation(
                                out=d.ap, in_=ident_b.ap, func=AF.Identity, scale=fcw.ap[:, l, tile, k:k + 1]),
                                reads=[ident_b, fcw], writes=[dg[h][k]])

            def conv_part(j):
                ub = upsb[j % 2]
                dg = diag[j % 2]
                pss = []
                for h in range(2):
                    u = ub[h]
                    ps2 = ps_next()
                    for k in range(3):
                        P.op("pe", lambda e, ps2=ps2, d=dg[h][k], u=u, k=k: e.matmul(
                            ps2.ap, lhsT=d.ap, rhs=u.ap[:, k:k + TT], start=(k == 0), stop=(k == 2)),
                            reads=[dg[h][k], u], writes=[ps2])
                    pss.append(ps2)
                sg = sgb[j % 2]
                P.op("act", lambda e, sg=sg, ps=pss[1], j=j: e.activation(
                    out=sg.ap, in_=ps.ap, func=AF.Silu, bias=fcb.ap[:, l, NFF + j:NFF + j + 1], scale=1.0),
                    reads=[pss[1], fcb], writes=[sg])
                P.op("dve", lambda e, sg=sg, ps=pss[0], j=j: e.scalar_tensor_tensor(
                    out=arena[j].ap, in0=ps.ap, scalar=fcb.ap[:, l, j:j + 1], in1=sg.ap,
                    op0=ALU.add, op1=ALU.mult), reads=[pss[0], fcb, sg], writes=[arena[j]])

            for j in range(NFF):
                up_part(j)
                if j >= 1:
                    conv_part(j - 1)
            conv_part(NFF - 1)
            if l == cfg.depth - 1:
                prefetch_x(t + 1)
            for mt in range(KT):
                ps = ps_next()
                for kh in range(2):
                    w = w_next(l, "dn", mt * 2 + kh)
                    wv = w.ap[:, 0:2816].rearrange("p (k c) -> p k c", c=128)
                    for k in range(22):
                        kk = kh * 22 + k
                        P.op("pe", lambda e, ps=ps, wv=wv, k=k, kk=kk: e.matmul(
                            ps.ap, lhsT=wv[:, k, :], rhs=arena[kk].ap, start=(kk == 0), stop=(kk == NFF - 1)),
                            reads=[w, arena[kk]], writes=[ps])
                P.op("dve", lambda e, ps=ps, mt=mt: e.tensor_tensor(out=xT[mt].ap, in0=ps.ap, in1=xT[mt].ap,
                                                                   op=ALU.add),
                     reads=[ps, xT[mt]], writes=[xT[mt]])

        ostage = [RA.view(c * 8192, [D], F32) for c in range(NCH)]

        def final_out(t):
            rmsnorm_stats(sqb, rstd)
            ytile = [RB.view(4096 + i * 2048, [TT], F32) for i in range(2)]
            for kt in range(KT):
                y = ytile[kt % 2]
                P.op("dve", lambda e, y=y, kt=kt: e.scalar_tensor_tensor(
                    out=y.ap, in0=xT[kt].ap, scalar=nw.ap[:, 2 * cfg.depth, kt:kt + 1], in1=rstd.ap,
                    op0=ALU.mult, op1=ALU.mult), reads=[xT[kt], nw, rstd], writes=[y])
                for c in range(NCH):
                    ps = ps_next()
                    P.op("pe", lambda e, ps=ps, y=y, c=c: e.transpose(
                        ps.ap[:, 0:128], y.ap[:, cs(c)], ident_f.ap), reads=[y, ident_f], writes=[ps])
                    og = ostage[c]
                    if c % 2 == 0:
                        P.op("dve", lambda e, ps=ps, og=og, kt=kt: e.tensor_copy(
                            out=og.ap[:, kt * 128:(kt + 1) * 128], in_=ps.ap[:, 0:128]), reads=[ps], writes=[og])
                    else:
                        P.op("act", lambda e, ps=ps, og=og, kt=kt: e.copy(
                            out=og.ap[:, kt * 128:(kt + 1) * 128], in_=ps.ap[:, 0:128]), reads=[ps], writes=[og])
            for c in range(NCH):
                r0 = t * TT + c * 128
                P.op("pool", lambda e, og=ostage[c], r0=r0: e.dma_start(out=out_d[r0:r0 + 128, :], in_=og.ap),
                     reads=[ostage[c]], dma=True)

        for t in range(cfg.ntile):
            load_x(t)
            for l in range(cfg.depth):
                if cfg.mixer:
                    mixer(l, t)
                if cfg.ffn:
                    ffn(l, t)
            final_out(t)
        P.emit(nc, st)
    return nc


def prep_shared(inputs, cfg):
    m = {}
    f32 = np.float32
    fmr, fmm, fms, tm_ret, tm_ml, gates = win_plan()
    for l in range(cfg.depth):
        w_in = np.asarray(inputs["w_in"][l], f32)
        m[f"w_fmr_{l}"] = strips_from_cols(w_in, fmr, 256)
        m[f"w_fmm_{l}"] = strips_from_cols(w_in, fmm, 256)
        m[f"w_fms_{l}"] = strips_from_cols(w_in, fms, 256)
        m[f"w_tmr_{l}"] = strips_from_cols(w_in, tm_ret, 256)
        m[f"w_tmm_{l}"] = strips_from_cols(w_in, tm_ml, 256)
        m[f"w_gt_{l}"] = strips_from_cols(w_in, gates, 16)
        m[f"w_glu_{l}"] = strips_from_cols(np.asarray(inputs["s5_glu_w"][l], f32), list(range(512)), 512)
        m[f"w_out_{l}"] = strips_from_cols(np.asarray(inputs["w_out"][l], f32), list(range(D)), 256)
        upc = []
        for j in range(NFF):
            upc += list(range(j * 128, (j + 1) * 128)) + list(range(D_FF + j * 128, D_FF + (j + 1) * 128))
        m[f"w_up_{l}"] = strips_from_cols(np.asarray(inputs["ffn_w_up"][l], f32), upc, 256)
        wd = np.asarray(inputs["ffn_w_down"][l], f32)
        dn = np.zeros((32, 128, 2816), f32)
        for mt in range(16):
            for kh in range(2):
                blk = wd[kh * 2816:(kh + 1) * 2816, mt * 128:(mt + 1) * 128]
                dn[mt * 2 + kh] = blk.reshape(22, 128, 128).transpose(1, 0, 2).reshape(128, 2816)
        m[f"w_dn_{l}"] = dn
    nwl = []
    for l in range(cfg.depth):
        nwl += [per_part(inputs["norm1_w"][l], KT), per_part(inputs["norm2_w"][l], KT)]
    nwl.append(per_part(inputs["final_norm_w"], KT))
    m["nw"] = np.ascontiguousarray(np.stack(nwl, axis=1))
    m["fcw"] = np.ascontiguousarray(np.stack(
        [np.stack([per_part(inputs["ffn_conv_w"][l][k], 88) for k in range(3)], axis=-1) for l in range(cfg.depth)],
        axis=1))
    m["fcb"] = np.ascontiguousarray(np.stack([per_part(inputs["ffn_conv_b"][l], 88) for l in range(cfg.depth)], axis=1))
    m["ident"] = np.eye(128, dtype=f32)
    t = np.arange(cfg.seq, dtype=f32)
    inv = (np.float32(10000.0) ** (-np.arange(0, 64, 2, dtype=f32) / np.float32(64))).astype(f32)
    ang = (t[:, None] * inv[None, :]).astype(f32)
    cosv, sinv = np.cos(ang).astype(f32), np.sin(ang).astype(f32)
    rc = np.zeros((128, cfg.seq), f32)
    rs = np.zeros((128, cfg.seq), f32)
    for r in range(128):
        d = r % 64
        rc[r] = cosv[:, d % 32]
        rs[r] = -sinv[:, d % 32] if d < 32 else sinv[:, d % 32]
    m["ropec"], m["ropes"] = rc, rs
    lg = np.log1p(-np.exp2(-5.0 - np.arange(6, dtype=f32))).astype(f32)
    idx = np.arange(128, dtype=f32)
    rtab = np.zeros((128, 12, 128), f32)
    for h in range(6):
        diff = idx[None, :] - idx[:, None]
        rtab[:, h, :] = np.where(diff >= 0, np.exp(np.maximum(diff, 0.0) * lg[h]), 0.0).astype(f32)
    for i in range(3):
        for r in range(128):
            h = 2 * i + r // 64
            rtab[r, 6 + i, :] = np.exp((idx + 1.0) * lg[h]).astype(f32)
        for col in range(128):
            h = 2 * i + col // 64
            rtab[:, 9 + i, col] = np.exp((127.0 - idx) * lg[h]).astype(f32)
    m["rtab"] = rtab
    m["tri"] = np.triu(np.ones((128, 128), f32))
    m["mcw"] = np.ascontiguousarray(np.stack(
        [np.stack([per_part(inputs["mlstm_conv_w"][l][k], 12) for k in range(4)], axis=-1) for l in range(cfg.depth)],
        axis=1))
    m["mcb"] = np.ascontiguousarray(np.stack([per_part(inputs["mlstm_conv_b"][l], 12) for l in range(cfg.depth)], axis=1))
    m["gbias"] = np.ascontiguousarray(np.asarray(inputs["mlstm_gate_b"], f32)[:cfg.depth])
    m["gnw"] = np.ascontiguousarray(np.stack(
        [np.concatenate([per_part(inputs["ret_gn_w"][l], 6), per_part(inputs["mlstm_gn_w"][l], 6)], axis=1)
         for l in range(cfg.depth)], axis=1))
    s5a = np.zeros((128, cfg.depth, 3, 16), f32)
    for l in range(cfg.depth):
        s5a[:, l, 0] = per_part(np.asarray(inputs["s5_A_re"][l], f32).reshape(-1), 16)
        s5a[:, l, 1] = per_part(np.asarray(inputs["s5_A_im"][l], f32).reshape(-1), 16)
        s5a[:, l, 2] = per_part(np.repeat(np.asarray(inputs["s5_log_step"][l], f32), 64), 16)
        sw = np.zeros((16, 128, 4, 128), f32)
        for ri, (bn, cn) in enumerate((("s5_B_re", "s5_C_re"), ("s5_B_im", "s5_C_im"))):
            Bm = np.asarray(inputs[bn][l], f32)
            Cm = np.asarray(inputs[cn][l], f32)
            for g in range(32):
                gl8 = g % 8
                tl = g // 2
                sw[tl, gl8 * 16:(gl8 + 1) * 16, ri, (g % 2) * 64:(g % 2) * 64 + 64] = Bm[g].T
                sw[tl, (g % 2) * 64:(g % 2) * 64 + 64, 2 + ri, gl8 * 16:(gl8 + 1) * 16] = Cm[g].T
        m[f"w_s5w_{l}"] = np.ascontiguousarray(sw.reshape(16, 128, 512))
    m["s5a"] = s5a
    m["s5d"] = np.ascontiguousarray(np.stack([per_part(inputs["s5_D"][l], 4) for l in range(cfg.depth)], axis=1))
    m["glub"] = np.ascontiguousarray(np.stack([per_part(inputs["s5_glu_b"][l], 4) for l in range(cfg.depth)], axis=1))
    m["jidx"] = np.ascontiguousarray(np.broadcast_to(np.arange(516, dtype=f32)[None, :], (128, 516)))
    return m


_CACHE = {}


def run(inputs, cfg, ncores):
    key = (cfg.seq, cfg.depth, cfg.mixer, cfg.ffn, cfg.parts)
    if key not in _CACHE:
        _CACHE[key] = build(cfg)
    nc = _CACHE[key]
    shared = prep_shared(inputs, cfg)
    x = np.asarray(inputs["x"], np.float32)
    in_maps = []
    for c in range(ncores):
        mm = dict(shared)
        mm["x"] = np.ascontiguousarray(x[c, :cfg.seq])
        in_maps.append(mm)
    res = run_bass_kernel_spmd(nc, in_maps, core_ids=list(range(ncores)))
    return np.stack([res.results[c]["out"] for c in range(ncores)], axis=0)


def kernel(**inputs):
    cfg = Cfg()
    return run(inputs, cfg, 8).astype(np.float32)
```
